# Optimizing a Trainium2 kernel written in Bass

```python
import math
import jax, jax.numpy as jnp
from jax import lax
import numpy as np

D_MODEL = 1024
BATCH = 8
SEQ = 2048
DEPTH = 2

GRID_W = 64
CTX_LEN = 256
EPS = 1e-6
CHUNK = 128
D_A = D_MODEL
G_A = 8
D_B = D_MODEL
CONV_W = 3
D_C = D_MODEL // 2
S5_GROUP = 16
G_C = D_C // S5_GROUP
S5_STATE = 64
N_HEADS = 8
QK_NOPE = 64
QK_ROPE = 32
QK_HEAD = QK_NOPE + QK_ROPE
V_HEAD = 64
Q_LORA = 768
KV_LORA = 256
AXIS_PAIRS = QK_ROPE // 4
ROPE_BASE = 10000.0
Q_BLOCK = 128
N_GROUPS = 4
EXPERTS_PER_GROUP = 8
N_EXPERTS = N_GROUPS * EXPERTS_PER_GROUP
TOP_K = 2
D_EXPERT = 512

D_IN_AB = 2 * D_A + 3 * D_B
D_IN_CD = D_C + Q_LORA + KV_LORA + QK_ROPE
D_MIX_AB = D_A + D_B
D_MIX_CD = D_C + N_HEADS * V_HEAD

kernel_name = 'hybrid_gmlp_conv_s5_mla_hmoe_dit'


def rms(x):
    xf = x.astype(jnp.float32)
    return (xf * lax.rsqrt(jnp.mean(xf * xf, axis=-1, keepdims=True) + EPS)).astype(x.dtype)


def modulate(x, shift, scale):
    return rms(x) * (1 + scale) + shift


def chunk_spatial_gating(u, v, sgu_norm, sgu_w, sgu_b):
    b, l, _ = v.shape
    vc = (rms(v) * sgu_norm).reshape(b, l // CHUNK, CHUNK, G_A, D_A // G_A)
    s = jnp.einsum('gpq,bnqgc->bnpgc', sgu_w, vc) + sgu_b.T[None, None, :, :, None]
    return u * s.reshape(b, l, D_A)


def short_conv(z, conv_w):
    return lax.conv_general_dilated(
        z, conv_w[:, None, :].astype(z.dtype), window_strides=(1,),
        padding=[(CONV_W // 2, CONV_W // 2)], dimension_numbers=('NWC', 'WIO', 'NWC'),
        feature_group_count=z.shape[-1])


def mixer_ab(h, w_in, sgu_norm, sgu_w, sgu_b, conv_w, w_out):
    z = h @ w_in
    u, v, gate_b, gate_c, xb = jnp.split(z, [D_A, 2 * D_A, 2 * D_A + D_B, 2 * D_A + 2 * D_B], axis=-1)
    y_a = chunk_spatial_gating(jax.nn.gelu(u), jax.nn.gelu(v), sgu_norm, sgu_w, sgu_b)
    y_b = gate_b * short_conv(gate_c * xb, conv_w)
    return jnp.concatenate([y_a, y_b], axis=-1) @ w_out


def cmul(ar, ai, br, bi):
    return ar * br - ai * bi, ar * bi + ai * br


def s5_discretise(a_re, a_im, log_dt, b_re, b_im):
    dt = jnp.exp(log_dt)[:, None]
    mag = jnp.exp(dt * a_re)
    ab_re, ab_im = mag * jnp.cos(dt * a_im), mag * jnp.sin(dt * a_im)
    den = a_re * a_re + a_im * a_im
    nr = ab_re - 1.0
    f_re = (nr * a_re + ab_im * a_im) / den
    f_im = (ab_im * a_re - nr * a_im) / den
    bb_re, bb_im = cmul(f_re[..., None], f_im[..., None], b_re, b_im)
    return ab_re, ab_im, bb_re, bb_im


def s5_scan(u, ab_re, ab_im, bb_re, bb_im, h0, reverse):
    bu_re = jnp.einsum('gpc,blgc->blgp', bb_re, u)
    bu_im = jnp.einsum('gpc,blgc->blgp', bb_im, u)
    if h0 is not None:
        first = -1 if reverse else 0
        i_re, i_im = cmul(ab_re, ab_im, h0[0], h0[1])
        bu_re = bu_re.at[:, first].add(i_re)
        bu_im = bu_im.at[:, first].add(i_im)
    shape = (1, u.shape[1]) + ab_re.shape
    a_re = jnp.broadcast_to(ab_re, shape)
    a_im = jnp.broadcast_to(ab_im, shape)

    def combine(e1, e2):
        a1r, a1i, b1r, b1i = e1
        a2r, a2i, b2r, b2i = e2
        ar, ai = cmul(a2r, a2i, a1r, a1i)
        br, bi = cmul(a2r, a2i, b1r, b1i)
        return ar, ai, br + b2r, bi + b2i

    _, _, h_re, h_im = lax.associative_scan(combine, (a_re, a_im, bu_re, bu_im), reverse=reverse, axis=1)
    return h_re, h_im


def s5_readout(c_re, c_im, h_re, h_im):
    return jnp.einsum('gcp,blgp->blgc', c_re, h_re) - jnp.einsum('gcp,blgp->blgc', c_im, h_im)


def s5_mixer(u_lat, u_ctx, a_re, a_im, log_dt, b_re, b_im, c_re, c_im, d_skip, w_glu, need_ctx_out):
    dtype = u_lat.dtype
    f = lambda t: t.astype(jnp.float32)
    ul = f(u_lat).reshape(u_lat.shape[0], u_lat.shape[1], G_C, S5_GROUP)
    uc = f(u_ctx).reshape(u_ctx.shape[0], u_ctx.shape[1], G_C, S5_GROUP)
    d = f(d_skip).reshape(G_C, S5_GROUP)
    y_l = ul * d
    y_c = uc * d if need_ctx_out else None
    for direction in range(2):
        rev = direction == 1
        ab_re, ab_im, bb_re, bb_im = s5_discretise(
            f(a_re[direction]), f(a_im[direction]), f(log_dt[direction]), f(b_re[direction]), f(b_im[direction]))
        hc_re, hc_im = s5_scan(uc, ab_re, ab_im, bb_re, bb_im, None, rev)
        end = 0 if rev else -1
        hl_re, hl_im = s5_scan(ul, ab_re, ab_im, bb_re, bb_im, (hc_re[:, end], hc_im[:, end]), rev)
        cr, ci = f(c_re[direction]), f(c_im[direction])
        y_l = y_l + s5_readout(cr, ci, hl_re, hl_im)
        if need_ctx_out:
            y_c = y_c + s5_readout(cr, ci, hc_re, hc_im)

    def glu(y):
        g = jax.nn.gelu(y.reshape(y.shape[0], y.shape[1], D_C)).astype(dtype)
        return g * jax.nn.sigmoid(g @ w_glu)

    return glu(y_l), (glu(y_c) if need_ctx_out else None)


def axial_angles(l):
    rows = l // GRID_W
    row = jnp.repeat(jnp.arange(rows, dtype=jnp.float32), GRID_W)
    col = jnp.tile(jnp.arange(GRID_W, dtype=jnp.float32), rows)
    inv_freq = ROPE_BASE ** (-jnp.arange(AXIS_PAIRS, dtype=jnp.float32) / AXIS_PAIRS)
    return row[:, None] * inv_freq, col[:, None] * inv_freq


def rotate(x, ang):
    cos = jnp.cos(ang)[None, :, None, :]
    sin = jnp.sin(ang)[None, :, None, :]
    xf = x.astype(jnp.float32)
    x1, x2 = xf[..., :AXIS_PAIRS], xf[..., AXIS_PAIRS:]
    return jnp.concatenate([x1 * cos - x2 * sin, x2 * cos + x1 * sin], axis=-1).astype(x.dtype)


def axial_rope(t, ang_row, ang_col):
    nope, r_row, r_col = jnp.split(t, [QK_NOPE, QK_NOPE + 2 * AXIS_PAIRS], axis=-1)
    return jnp.concatenate([nope, rotate(r_row, ang_row), rotate(r_col, ang_col)], axis=-1)


def mla_q(cq, q_norm, w_uq, qn_gain, angles):
    b, l, _ = cq.shape
    q = ((rms(cq) * q_norm) @ w_uq).reshape(b, l, N_HEADS, QK_HEAD)
    q = rms(q) * qn_gain
    return axial_rope(q, *angles) if angles is not None else q


def mla_kv(ckv, k_rope, kv_norm, w_uk, w_uv, kn_gain, angles):
    b, l, _ = ckv.shape
    ckv_n = rms(ckv) * kv_norm
    k_nope = (ckv_n @ w_uk).reshape(b, l, N_HEADS, QK_NOPE)
    v = (ckv_n @ w_uv).reshape(b, l, N_HEADS, V_HEAD)
    k_r = jnp.broadcast_to(k_rope[:, :, None, :], (b, l, N_HEADS, QK_ROPE))
    k = rms(jnp.concatenate([k_nope, k_r], axis=-1)) * kn_gain
    k = axial_rope(k, *angles) if angles is not None else k
    return k, v


def attend(q, k, v):
    s = jnp.einsum('bqhd,bkhd->bhqk', q, k).astype(jnp.float32) * (QK_HEAD ** -0.5)
    p = jax.nn.softmax(s, axis=-1).astype(v.dtype)
    return jnp.einsum('bhqk,bkhd->bqhd', p, v)


def blockwise_attend(q, k, v):
    b, l, h, d = q.shape
    qb = q.reshape(b, l // Q_BLOCK, Q_BLOCK, h, d).transpose(1, 0, 2, 3, 4)
    ob = lax.map(lambda qi: attend(qi, k, v), qb)
    return ob.transpose(1, 0, 2, 3, 4).reshape(b, l, h, v.shape[-1])


def mixer_cd(h, hc, w_in, a_re, a_im, log_dt, b_re, b_im, c_re, c_im, d_skip, w_glu,
             q_norm, kv_norm, w_uq, w_uk, w_uv, qn_gain, kn_gain, w_out, angles, need_ctx_out):
    idx = [D_C, D_C + Q_LORA, D_C + Q_LORA + KV_LORA]
    u_l, cq_l, ckv_l, kr_l = jnp.split(h @ w_in, idx, axis=-1)
    u_c, cq_c, ckv_c, kr_c = jnp.split(hc @ w_in, idx, axis=-1)
    s5_l, s5_c = s5_mixer(u_l, u_c, a_re, a_im, log_dt, b_re, b_im, c_re, c_im, d_skip, w_glu, need_ctx_out)
    k_c, v_c = mla_kv(ckv_c, kr_c, kv_norm, w_uk, w_uv, kn_gain, None)
    k_l, v_l = mla_kv(ckv_l, kr_l, kv_norm, w_uk, w_uv, kn_gain, angles)
    q_l = mla_q(cq_l, q_norm, w_uq, qn_gain, angles)
    o_l = blockwise_attend(q_l, jnp.concatenate([k_c, k_l], axis=1), jnp.concatenate([v_c, v_l], axis=1))
    b, l = h.shape[:2]
    y_l = jnp.concatenate([s5_l, o_l.reshape(b, l, N_HEADS * V_HEAD)], axis=-1) @ w_out
    if not need_ctx_out:
        return y_l, None
    q_c = mla_q(cq_c, q_norm, w_uq, qn_gain, None)
    o_c = attend(q_c, k_c, v_c)
    y_c = jnp.concatenate([s5_c, o_c.reshape(b, hc.shape[1], N_HEADS * V_HEAD)], axis=-1) @ w_out
    return y_l, y_c


def hier_moe(x, w_grp, b_grp, w_exp, b_exp, w_gate, w_up, w_down):
    shape = x.shape
    t = x.reshape(-1, shape[-1])
    n = t.shape[0]
    p_grp = jax.nn.softmax((t @ w_grp).astype(jnp.float32) + b_grp.astype(jnp.float32), axis=-1)
    p_top, grp = lax.top_k(p_grp, 1)
    e_logits = ((t @ w_exp).astype(jnp.float32) + b_exp.astype(jnp.float32)).reshape(n, N_GROUPS, EXPERTS_PER_GROUP)
    sel = jnp.take_along_axis(e_logits, jnp.broadcast_to(grp[:, :, None], (n, 1, EXPERTS_PER_GROUP)), axis=1)[:, 0]
    top_l, top_i = lax.top_k(sel, TOP_K)
    w_top = jax.nn.softmax(top_l, axis=-1) * p_top
    eid = grp * EXPERTS_PER_GROUP + top_i
    gates = jnp.sum(jax.nn.one_hot(eid, N_EXPERTS, dtype=jnp.float32) * w_top[..., None], axis=1).astype(t.dtype)
    y = jnp.zeros_like(t)
    for e in range(N_EXPERTS):
        he = jax.nn.silu(t @ w_gate[e]) * (t @ w_up[e])
        y = y + gates[:, e:e + 1] * (he @ w_down[e])
    return y.reshape(shape)


def setup_inputs(seed: int = 0) -> dict:
    key = jax.random.key(seed)
    ks = iter(jax.random.split(key, 48))
    nrm = lambda shape, scale: scale * jax.random.normal(next(ks), shape, jnp.float32)
    ne, no = (DEPTH + 1) // 2, DEPTH // 2
    D = D_MODEL
    n_idx = jnp.arange(S5_STATE, dtype=jnp.float32)
    return {
        'x': nrm((BATCH, SEQ, D), 1.0),
        'c': nrm((BATCH, D), 1.0),
        'ctx': nrm((BATCH, CTX_LEN, D), 1.0),
        'c_ctx': nrm((D,), 1.0),
        'ada_w': nrm((DEPTH, D, 6 * D), 0.5 * D ** -0.5),
        'ada_b': nrm((DEPTH, 6 * D), 0.02),
        'ab_w_in': nrm((ne, D, D_IN_AB), D ** -0.5),
        'sgu_norm': 1.0 + nrm((ne, D_A), 0.02),
        'sgu_w': nrm((ne, G_A, CHUNK, CHUNK), CHUNK ** -0.5),
        'sgu_b': 1.0 + nrm((ne, G_A, CHUNK), 0.02),
        'conv_w': nrm((ne, CONV_W, D_B), CONV_W ** -0.5),
        'ab_w_out': nrm((ne, D_MIX_AB, D), D_MIX_AB ** -0.5),
        'cd_w_in': nrm((no, D, D_IN_CD), D ** -0.5),
        's5_a_re': -0.5 + nrm((no, 2, G_C, S5_STATE), 0.01),
        's5_a_im': math.pi * n_idx + nrm((no, 2, G_C, S5_STATE), 0.01),
        's5_log_dt': jax.random.uniform(next(ks), (no, 2, G_C), jnp.float32, math.log(1e-3), math.log(1e-1)),
        's5_b_re': nrm((no, 2, G_C, S5_STATE, S5_GROUP), (2 * S5_GROUP) ** -0.5),
        's5_b_im': nrm((no, 2, G_C, S5_STATE, S5_GROUP), (2 * S5_GROUP) ** -0.5),
        's5_c_re': nrm((no, 2, G_C, S5_GROUP, S5_STATE), (S5_STATE / 2) ** -0.5),
        's5_c_im': nrm((no, 2, G_C, S5_GROUP, S5_STATE), (S5_STATE / 2) ** -0.5),
        's5_d': nrm((no, D_C), 1.0),
        's5_w_glu': nrm((no, D_C, D_C), D_C ** -0.5),
        'mla_q_norm': 1.0 + nrm((no, Q_LORA), 0.02),
        'mla_kv_norm': 1.0 + nrm((no, KV_LORA), 0.02),
        'mla_w_uq': nrm((no, Q_LORA, N_HEADS * QK_HEAD), Q_LORA ** -0.5),
        'mla_w_uk': nrm((no, KV_LORA, N_HEADS * QK_NOPE), KV_LORA ** -0.5),
        'mla_w_uv': nrm((no, KV_LORA, N_HEADS * V_HEAD), KV_LORA ** -0.5),
        'mla_qn_gain': 1.0 + nrm((no, QK_HEAD), 0.02),
        'mla_kn_gain': 1.0 + nrm((no, QK_HEAD), 0.02),
        'cd_w_out': nrm((no, D_MIX_CD, D), D_MIX_CD ** -0.5),
        'moe_w_grp': nrm((DEPTH, D, N_GROUPS), D ** -0.5),
        'moe_b_grp': nrm((DEPTH, N_GROUPS), 0.01),
        'moe_w_exp': nrm((DEPTH, D, N_EXPERTS), D ** -0.5),
        'moe_b_exp': nrm((DEPTH, N_EXPERTS), 0.01),
        'moe_w_gate': nrm((DEPTH, N_EXPERTS, D, D_EXPERT), D ** -0.5),
        'moe_w_up': nrm((DEPTH, N_EXPERTS, D, D_EXPERT), D ** -0.5),
        'moe_w_down': nrm((DEPTH, N_EXPERTS, D_EXPERT, D), D_EXPERT ** -0.5),
    }


def reference(x, c, ctx, c_ctx, ada_w, ada_b, ab_w_in, sgu_norm, sgu_w, sgu_b, conv_w, ab_w_out,
              cd_w_in, s5_a_re, s5_a_im, s5_log_dt, s5_b_re, s5_b_im, s5_c_re, s5_c_im, s5_d, s5_w_glu,
              mla_q_norm, mla_kv_norm, mla_w_uq, mla_w_uk, mla_w_uv, mla_qn_gain, mla_kn_gain, cd_w_out,
              moe_w_grp, moe_b_grp, moe_w_exp, moe_b_exp, moe_w_gate, moe_w_up, moe_w_down):
    angles = axial_angles(x.shape[1])
    xl, xc = x, ctx
    for i in range(DEPTH):
        last = i == DEPTH - 1
        j = i // 2
        mod = (jax.nn.silu(c) @ ada_w[i] + ada_b[i])[:, None, :]
        mod_c = jax.nn.silu(c_ctx) @ ada_w[i] + ada_b[i]
        sh1, sc1, g1, sh2, sc2, g2 = jnp.split(mod, 6, axis=-1)
        csh1, csc1, cg1, csh2, csc2, cg2 = jnp.split(mod_c, 6, axis=-1)
        h = modulate(xl, sh1, sc1)
        hc = modulate(xc, csh1, csc1)
        if i % 2 == 0:
            y = mixer_ab(h, ab_w_in[j], sgu_norm[j], sgu_w[j], sgu_b[j], conv_w[j], ab_w_out[j])
            yc = None if last else mixer_ab(hc, ab_w_in[j], sgu_norm[j], sgu_w[j], sgu_b[j], conv_w[j], ab_w_out[j])
        else:
            y, yc = mixer_cd(h, hc, cd_w_in[j], s5_a_re[j], s5_a_im[j], s5_log_dt[j], s5_b_re[j], s5_b_im[j],
                             s5_c_re[j], s5_c_im[j], s5_d[j], s5_w_glu[j], mla_q_norm[j], mla_kv_norm[j],
                             mla_w_uq[j], mla_w_uk[j], mla_w_uv[j], mla_qn_gain[j], mla_kn_gain[j], cd_w_out[j],
                             angles, not last)
        xl = xl + g1 * y
        xl = xl + g2 * hier_moe(modulate(xl, sh2, sc2), moe_w_grp[i], moe_b_grp[i], moe_w_exp[i], moe_b_exp[i],
                                moe_w_gate[i], moe_w_up[i], moe_w_down[i])
        if not last:
            xc = xc + cg1 * yc
            xc = xc + cg2 * hier_moe(modulate(xc, csh2, csc2), moe_w_grp[i], moe_b_grp[i], moe_w_exp[i],
                                     moe_b_exp[i], moe_w_gate[i], moe_w_up[i], moe_w_down[i])
    return xl
```

```python
import math
from contextlib import ExitStack
from types import SimpleNamespace

import numpy as np
import concourse.bass as bass
import concourse.mybir as mybir
from concourse.bass_utils import run_bass_kernel_spmd

F32 = mybir.dt.float32
BF16 = mybir.dt.bfloat16
I32 = mybir.dt.int32
AF = mybir.ActivationFunctionType
ALU = mybir.AluOpType

D = 1024
SEQ = 2048
CTX = 256
NT_L = 16
NT = 18
T_ALL = NT * 128
EPS = 1e-6
NEXP = 32
DEXP = 512
BIG = 1.0e30


_UID = [0]


def _sb(nc, name, shape, dt):
    _UID[0] += 1
    return nc.sbuf_tensor("%s_u%d" % (name, _UID[0]), shape, dt)


def _ps(nc, name, shape, dt):
    _UID[0] += 1
    return nc.psum_tensor("%s_u%d" % (name, _UID[0]), shape, dt)


class _Eng:
    def __init__(self, name, sem):
        self.name = name
        self.sem = sem
        self.tick = 0
        self.ins = []
        self.sim = []
        self.waited = {}


class Prog:
    def __init__(self, nc, enter):
        self.nc = nc
        self.enter = enter
        self.eng = {n: _Eng(n, enter(nc.semaphore("s_" + n))) for n in ("pe", "act", "dve", "pool", "sp")}
        self.res = {}
        self.dsem = {}
        self.ps_i = 0

    def _wait(self, X, ev):
        if ev is None:
            return
        kind, k, v = ev
        key = (kind, k)
        if X.waited.get(key, 0) >= v:
            return
        X.waited[key] = v
        sem = self.eng[k].sem if kind == "e" else self.dsem[k][0]
        X.ins.append(lambda e, sem=sem, v=v: e.wait_ge(sem, v))
        X.sim.append(("w", key, v))

    def _deps(self, X, reads, writes):
        for r in reads:
            st = self.res.get(r)
            if st:
                self._wait(X, st[0])
        for w in writes:
            st = self.res.get(w)
            if st:
                self._wait(X, st[0])
                for ev in st[1].values():
                    self._wait(X, ev)

    def _commit(self, ev, reads, writes, rkey):
        for r in reads:
            st = self.res.setdefault(r, [None, {}])
            st[1][rkey] = ev
        for w in writes:
            self.res[w] = [ev, {}]

    def op(self, en, fn, reads=(), writes=()):
        X = self.eng[en]
        self._deps(X, reads, writes)
        X.tick += 1
        sem = X.sem
        X.ins.append(lambda e, fn=fn, sem=sem: fn(e).then_inc(sem, 1))
        X.sim.append(("i", ("e", en), 1))
        self._commit(("e", en, X.tick), reads, writes, en)

    def act(self, fn, reads=(), writes=()):
        self.op("act", fn, reads, writes)

    def dve(self, fn, reads=(), writes=()):
        self.op("dve", fn, reads, writes)

    def pool(self, fn, reads=(), writes=()):
        self.op("pool", fn, reads, writes)

    def mm(self, out, pairs, reads=(), writes=()):
        X = self.eng["pe"]
        self._deps(X, reads, writes)
        n = len(pairs)
        sem = X.sem
        for i, (l, r) in enumerate(pairs):
            if i == n - 1:
                X.ins.append(lambda e, l=l, r=r, i=i: e.matmul(out, l, r, start=(i == 0), stop=True).then_inc(sem, 1))
            else:
                X.ins.append(lambda e, l=l, r=r, i=i: e.matmul(out, l, r, start=(i == 0), stop=False))
        X.tick += 1
        X.sim.append(("i", ("e", "pe"), 1))
        self._commit(("e", "pe", X.tick), reads, writes, "pe")

    def tr(self, out, in_, ident, reads=(), writes=()):
        self.op("pe", lambda e: e.transpose(out, in_, ident), reads, writes)

    def dma(self, q, out, in_, reads=(), writes=(), semkey=None, **kw):
        X = self.eng[q]
        self._deps(X, reads, writes)
        if semkey not in self.dsem:
            self.dsem[semkey] = [self.enter(self.nc.semaphore("d%d" % len(self.dsem))), 0]
        d = self.dsem[semkey]
        d[1] += 16
        sem = d[0]
        X.ins.append(lambda e: e.dma_start(out=out, in_=in_, **kw).then_inc(sem, 16))
        X.sim.append(("i", ("d", semkey), 16))
        self._commit(("d", semkey, d[1]), reads, writes, ("d", semkey))

    def barrier(self):
        evs = [("e", n, E.tick) for n, E in self.eng.items() if E.tick > 0]
        evs += [("d", k, d[1]) for k, d in self.dsem.items()]
        for X in self.eng.values():
            for ev in evs:
                self._wait(X, ev)
        self.res = {}

    def simulate(self):
        if not hasattr(self, "semval"):
            self.semval = {}
        pos = {n: 0 for n in self.eng}
        while True:
            prog = False
            for n, E in self.eng.items():
                while pos[n] < len(E.sim):
                    kind, key, v = E.sim[pos[n]]
                    if kind == "w":
                        if self.semval.get(key, 0) < v:
                            break
                    else:
                        self.semval[key] = self.semval.get(key, 0) + v
                    pos[n] += 1
                    prog = True
            if not prog:
                break
        stuck = {n: (pos[n], len(E.sim), E.sim[pos[n]], self.semval.get(E.sim[pos[n]][1], 0))
                 for n, E in self.eng.items() if pos[n] < len(E.sim)}
        if stuck:
            raise RuntimeError("DEADLOCK in recorded program: %r" % (stuck,))
        for E in self.eng.values():
            E.sim = []

    def flush(self):
        self.simulate()
        with self.nc.Block() as blk:
            def mk(name):
                lst = self.eng[name].ins

                def run(e):
                    for f in lst:
                        f(e)
                return run
            blk.tensor(mk("pe"))
            blk.scalar(mk("act"))
            blk.vector(mk("dve"))
            blk.gpsimd(mk("pool"))
            blk.sync(mk("sp"))
        for E in self.eng.values():
            E.ins = []


def run_zip(gens, width):
    pend = list(gens)
    act = []
    while pend or act:
        while pend and len(act) < width:
            act.append(pend.pop(0))
        for g in list(act):
            try:
                next(g)
            except StopIteration:
                act.remove(g)


def rows_view(ap2d):
    return ap2d.rearrange("(k p) n -> p k n", p=128)


def phase_init(P, G):
    nc = P.nc
    P.dma("sp", G.xl[:, 0:NT_L, :], G.x.rearrange("(t p) d -> p t d", p=128),
          writes=[("xl", t) for t in range(NT_L)], semkey="ldx")
    P.dma("sp", G.xl[:, NT_L:NT, :], G.ctx.rearrange("(t p) d -> p t d", p=128),
          writes=[("xl", t) for t in range(NT_L, NT)], semkey="ldc")
    P.pool(lambda e: e.iota(G.iot[:, 0:128], [[1, 128]], base=0, channel_multiplier=-1), writes=["iot"])
    P.pool(lambda e: e.tensor_single_scalar(out=G.ident[:], in_=G.iot[:, 0:128], scalar=0, op=ALU.is_equal),
           reads=["iot"], writes=["ident"])
    P.pool(lambda e: e.memset(G.ones[:], 1.0), writes=["ones"])
    P.pool(lambda e: e.memset(G.onesb[:], 1.0), writes=["onesb"])


def phase_ada(P, G, i, blocks):
    nc = P.nc
    with ExitStack() as ph:
        A = ph.enter_context
        wblk = [A(_sb(nc, "adaw%d" % s, [128, 8, 512], F32)) for s in range(2)]
        brow = [A(_sb(nc, "adab%d" % s, [1, 512], F32)) for s in range(2)]
        crow = A(_sb(nc, "crow", [2, 1024], F32))
        scT = A(_sb(nc, "scT", [128, 8, 2], F32))
        rowb = A(_sb(nc, "rowb", [1, 2, 512], F32))
        pst = [A(_ps(nc, "psa%d" % s, [128, 512], F32)) for s in range(4)]
        P.dma("sp", crow[0:1, :], G.c[0:1, :], writes=["crow"], semkey="crow")
        P.dma("sp", crow[1:2, :], G.c_ctx[0:1, :], writes=["crow"], semkey="crow")
        P.act(lambda e: e.activation(out=crow[:], in_=crow[:], func=AF.Silu), reads=["crow"], writes=["crow"])
        for k in range(8):
            P.mm(pst[0][:, 2 * k:2 * k + 2], [(crow[0:2, k * 128:(k + 1) * 128], G.ident[0:2, 0:2])],
                 reads=["crow", "ident"], writes=[("psa", 0)])
        P.dve(lambda e: e.tensor_copy(out=scT[:].rearrange("p k r -> p (k r)"), in_=pst[0][:, 0:16]),
              reads=[("psa", 0)], writes=["scT"])
        pi = 1
        for bi, blk in enumerate(blocks):
            s = bi % 2
            which, half = blk // 2, blk % 2
            P.dma("sp", wblk[s][:], rows_view(G.ada_w[i, :, blk * 512:(blk + 1) * 512]),
                  writes=[("adaw", s)], semkey="adaw%d" % s)
            P.dma("sp", brow[s][:], G.ada_b[i:i + 1, blk * 512:(blk + 1) * 512],
                  writes=[("adab", s)], semkey="adab%d" % s)
            if which in (2, 5):
                gt = G.g1b if which == 2 else G.g2b
                for r in range(2):
                    pk = pi % 4
                    pi += 1
                    pairs = [(scT[:, k, r:r + 1], wblk[s][:, k, :]) for k in range(8)]
                    pairs.append((G.ones[0:1, 0:1], brow[s][0:1, :]))
                    P.mm(pst[pk][0:1, :], pairs, reads=["scT", ("adaw", s), ("adab", s), "ones"], writes=[("psa", pk)])
                    P.dve(lambda e, pk=pk, r=r: e.tensor_copy(out=rowb[0:1, r, :], in_=pst[pk][0:1, :]),
                          reads=[("psa", pk)], writes=[("rowb", r)])
                    pk2 = pi % 4
                    pi += 1
                    P.mm(pst[pk2][:, :], [(G.ones[0:1, 0:128], rowb[0:1, r, :])],
                         reads=[("rowb", r), "ones"], writes=[("psa", pk2)])
                    P.act(lambda e, pk2=pk2, r=r, gt=gt, half=half: e.activation(
                        out=gt[:, r, half * 512:(half + 1) * 512], in_=pst[pk2][:, :], func=AF.Identity),
                        reads=[("psa", pk2)], writes=[("gt", which, r)])
            else:
                for f in range(4):
                    pk = pi % 4
                    pi += 1
                    pairs = [(wblk[s][:, k, f * 128:(f + 1) * 128], scT[:, k, :]) for k in range(8)]
                    pairs.append((brow[s][0:1, f * 128:(f + 1) * 128], G.ones[0:1, 0:2]))
                    P.mm(pst[pk][:, 0:2], pairs, reads=["scT", ("adaw", s), ("adab", s), "ones"], writes=[("psa", pk)])
                    addc = 1.0 if which in (1, 4) else 0.0
                    P.dve(lambda e, pk=pk, f=f, which=which, half=half, addc=addc: e.tensor_scalar(
                        out=G.modT[:, which, half * 4 + f, :], in0=pst[pk][:, 0:2], scalar1=addc, scalar2=None,
                        op0=ALU.add), reads=[("psa", pk)], writes=["modT"])
        P.barrier()
        P.flush()


def modulate(P, G, hT, wsh, wsc, ntiles, scr, router=None):
    def tile_gen(t):
        r = 0 if t < NT_L else 1
        s = t % 2
        xn = scr.xn[s]
        P.act(lambda e: e.activation(out=scr.junk[s][:], in_=G.xl[:, t, :], func=AF.Square,
                                     accum_out=scr.st[:, t, 0:1]),
              reads=[("xl", t)], writes=[("junk", s), ("st", t)])
        yield
        P.dve(lambda e: e.tensor_scalar(out=scr.st[:, t, 1:2], in0=scr.st[:, t, 0:1], scalar1=1.0 / D,
                                        scalar2=EPS, op0=ALU.mult, op1=ALU.add),
              reads=[("st", t)], writes=[("st1", t)])
        yield
        P.act(lambda e: e.activation(out=scr.st[:, t, 2:3], in_=scr.st[:, t, 1:2], func=AF.Sqrt),
              reads=[("st1", t)], writes=[("st2", t)])
        yield
        P.dve(lambda e: e.reciprocal(out=scr.st[:, t, 3:4], in_=scr.st[:, t, 2:3]),
              reads=[("st2", t)], writes=[("st3", t)])
        yield
        P.dve(lambda e: e.tensor_scalar(out=xn[:], in0=G.xl[:, t, :], scalar1=scr.st[:, t, 3:4],
                                        scalar2=None, op0=ALU.mult),
              reads=[("xl", t), ("st3", t)], writes=[("xn", s)])
        yield
        for hb in range(2):
            pk, ps = scr.next_ps()
            for j in range(4):
                c = hb * 4 + j
                P.tr(ps[:, j * 128:(j + 1) * 128], xn[:, c * 128:(c + 1) * 128], G.ident[:],
                     reads=[("xn", s), "ident"], writes=[pk])
            yield
            for j in range(4):
                c = hb * 4 + j
                if router is None:
                    dst = hT[:, c, t * 128:(t + 1) * 128]
                    wr = [("hT", t, c)]
                else:
                    dst = scr.t32[s][:, c, :]
                    wr = [("t32", s, c)]
                P.act(lambda e, dst=dst, ps=ps, j=j, c=c: e.activation(
                    out=dst, in_=ps[:, j * 128:(j + 1) * 128], func=AF.Identity,
                    scale=G.modT[:, wsc, c, r:r + 1], bias=G.modT[:, wsh, c, r:r + 1]),
                    reads=[pk, "modT"], writes=wr)
            yield
        if router is not None:
            P.pool(lambda e: e.tensor_copy(out=hT[:, :, t * 128:(t + 1) * 128], in_=scr.t32[s][:]),
                   reads=[("t32", s, c) for c in range(8)], writes=[("hT", t, c) for c in range(8)])
            yield
            router(t, s)
            yield
    run_zip([tile_gen(t) for t in range(ntiles)], 2)


def make_scr(P, A, nps, with_t32=False):
    nc = P.nc
    scr = SimpleNamespace()
    scr.junk = [A(_sb(nc, "junk%d" % s, [128, 1024], BF16)) for s in range(2)]
    scr.xn = [A(_sb(nc, "xn%d" % s, [128, 1024], F32)) for s in range(2)]
    scr.st = A(_sb(nc, "mstat", [128, NT, 4], F32))
    if with_t32:
        scr.t32 = [A(_sb(nc, "t32_%d" % s, [128, 8, 128], F32)) for s in range(2)]
    scr.ps = [A(_ps(nc, "psm%d" % s, [128, 512], F32)) for s in range(nps)]
    scr.i = 0

    def next_ps():
        k = scr.i % nps
        scr.i += 1
        return ("psm", k), scr.ps[k]
    scr.next_ps = next_ps
    return scr


def resid_update(P, G, t, ps, pk, gt, c0, n, tmp, tk, gate=None):
    r = 0 if t < NT_L else 1
    if gate is None:
        P.dve(lambda e: e.tensor_tensor(out=tmp[:, 0:n], in0=ps, in1=gt[:, r, c0:c0 + n], op=ALU.mult),
              reads=[pk, ("gt", r)], writes=[tk])
    else:
        P.dve(lambda e: e.scalar_tensor_tensor(out=tmp[:, 0:n], in0=ps, scalar=gate, in1=gt[:, r, c0:c0 + n],
                                               op0=ALU.mult, op1=ALU.mult),
              reads=[pk, ("gt", r), "gates"], writes=[tk])
    P.dve(lambda e: e.tensor_tensor(out=G.xl[:, t, c0:c0 + n], in0=G.xl[:, t, c0:c0 + n], in1=tmp[:, 0:n],
                                    op=ALU.add),
          reads=[tk, ("xl", t)], writes=[("xl", t)])


TOKBLK = [(0, 512), (512, 512), (1024, 512), (1536, 512), (2048, 256)]


def phase_mixer_ab(P, G):
    nc = P.nc
    with ExitStack() as ph:
        A = ph.enter_context
        hT = A(_sb(nc, "hT", [128, 8, T_ALL], BF16))
        ntb = 5
        with ExitStack() as p1:
            scr = make_scr(P, p1.enter_context, 4)
            modulate(P, G, hT, 0, 1, NT, scr)
            P.barrier()
            P.flush()
        with ExitStack() as p2:
            B = p2.enter_context
            ybT = B(_sb(nc, "ybT", [128, 8, T_ALL], BF16))
            wB = [B(_sb(nc, "wB%d" % s, [128, 8, 3, 128], BF16)) for s in range(2)]
            prod = B(_sb(nc, "prod", [128, 2308], F32))
            tmpc = [B(_sb(nc, "tmpc%d" % s, [128, 512], F32)) for s in range(2)]
            cv = [B(_sb(nc, "cv%d" % s, [128, 512], F32)) for s in range(2)]
            cw3 = prod[0:3, 0:1024]
            cwT = B(_sb(nc, "cwT", [128, 8, 3], F32))
            woB = B(_sb(nc, "woB", [128, 8, 1024], BF16))
            tmpr = [B(_sb(nc, "tmpr%d" % s, [128, 512], F32)) for s in range(2)]
            pss = [B(_ps(nc, "psb%d" % s, [128, 512], F32)) for s in range(8)]
            psi = [0]

            def nps():
                k = psi[0] % 8
                psi[0] += 1
                return ("psb", k), pss[k]
            P.dma("sp", cw3, G.conv_w[0, :, :], writes=["prod"], semkey="cw3")
            pk, ps = nps()
            for c in range(8):
                P.tr(ps[:, c * 3:c * 3 + 3], prod[0:3, c * 128:(c + 1) * 128], G.ident[0:3, 0:3],
                     reads=["prod", "ident"], writes=[pk])
            P.dve(lambda e: e.tensor_copy(out=cwT[:].rearrange("p c k -> p (c k)"), in_=ps[:, 0:24]),
                  reads=[pk], writes=["cwT"])
            P.dma("pool", woB[:], rows_view(G.ab_w_out[0, 1024:2048, :]), writes=["woB"], semkey="woB")
            P.dve(lambda e: e.memset(prod[:], 0.0), reads=["cwT"], writes=["prod"])

            def poff(a):
                return 1 + a if a < SEQ else 2051 + (a - SEQ)
            for c in range(8):
                s = c % 2
                for j in range(3):
                    col = 2048 + j * 1024 + c * 128
                    P.dma("pool", wB[s][:, :, j, :], rows_view(G.ab_w_in[0, :, col:col + 128]),
                          writes=[("wB", s, j)], semkey="wB%d_%d" % (s, j))
                for bi, (a, n) in enumerate(TOKBLK):
                    pkc, psc = nps()
                    P.mm(psc[:, 0:n], [(wB[s][:, k, 1, :], hT[:, k, a:a + n]) for k in range(8)],
                         reads=[("wB", s, 1), "hTall"], writes=[pkc])
                    pkx, psx = nps()
                    P.mm(psx[:, 0:n], [(wB[s][:, k, 2, :], hT[:, k, a:a + n]) for k in range(8)],
                         reads=[("wB", s, 2), "hTall"], writes=[pkx])
                    ts = bi % 2
                    P.act(lambda e, ts=ts, psc=psc, n=n: e.activation(out=tmpc[ts][:, 0:n], in_=psc[:, 0:n],
                                                                      func=AF.Identity),
                          reads=[pkc], writes=[("tmpc", ts)])
                    o = poff(a)
                    P.dve(lambda e, ts=ts, psx=psx, n=n, o=o: e.tensor_tensor(
                        out=prod[:, o:o + n], in0=tmpc[ts][:, 0:n], in1=psx[:, 0:n], op=ALU.mult),
                        reads=[("tmpc", ts), pkx], writes=["prod"])
                for bi, (a, n) in enumerate(TOKBLK):
                    pkb, psb = nps()
                    P.mm(psb[:, 0:n], [(wB[s][:, k, 0, :], hT[:, k, a:a + n]) for k in range(8)],
                         reads=[("wB", s, 0), "hTall"], writes=[pkb])
                    ts = bi % 2
                    o = poff(a)
                    P.act(lambda e, ts=ts, n=n, o=o, c=c: e.activation(
                        out=cv[ts][:, 0:n], in_=prod[:, o - 1:o - 1 + n], func=AF.Identity,
                        scale=cwT[:, c, 0:1]), reads=["prod", "cwT"], writes=[("cv", ts)])
                    for kk in (1, 2):
                        P.dve(lambda e, ts=ts, n=n, o=o, c=c, kk=kk: e.scalar_tensor_tensor(
                            out=cv[ts][:, 0:n], in0=prod[:, o - 1 + kk:o - 1 + kk + n], scalar=cwT[:, c, kk:kk + 1],
                            in1=cv[ts][:, 0:n], op0=ALU.mult, op1=ALU.add),
                            reads=["prod", "cwT", ("cv", ts)], writes=[("cv", ts)])
                    P.dve(lambda e, ts=ts, n=n, a=a, c=c, psb=psb: e.tensor_tensor(
                        out=ybT[:, c, a:a + n], in0=cv[ts][:, 0:n], in1=psb[:, 0:n], op=ALU.mult),
                        reads=[("cv", ts), pkb], writes=["ybT"])
            ui = 0
            for t in range(NT):
                for db in range(2):
                    pky, psy = nps()
                    P.mm(psy[:, :], [(ybT[:, k, t * 128:(t + 1) * 128], woB[:, k, db * 512:(db + 1) * 512])
                                     for k in range(8)], reads=["ybT", "woB"], writes=[pky])
                    resid_update(P, G, t, psy[:, :], pky, G.g1b, db * 512, 512, tmpr[ui % 2], ("tmpr", ui % 2))
                    ui += 1
            P.barrier()
            P.flush()
        with ExitStack() as p3:
            B = p3.enter_context
            wU = B(_sb(nc, "wU", [128, 8, 1024], BF16))
            wV = B(_sb(nc, "wV", [128, 8, 1024], BF16))
            woA = B(_sb(nc, "woA", [128, 8, 1024], BF16))
            guT = B(_sb(nc, "guT", [128, 8, 512], BF16))
            gv = B(_sb(nc, "gv", [128, 1024], F32))
            vc = [B(_sb(nc, "vc%d" % s, [128, 1024], BF16)) for s in range(2)]
            yaT = [B(_sb(nc, "yaT%d" % s, [128, 8, 128], BF16)) for s in range(2)]
            sgwT = B(_sb(nc, "sgwT", [128, 8, 128], BF16))
            biasB = B(_sb(nc, "biasB", [128, 1024], F32))
            normB = B(_sb(nc, "normB", [128, 1024], F32))
            sgw = normB[:].rearrange("p (g q) -> p g q", g=8)
            sjunk = B(_sb(nc, "sjunk", [128, 1024], BF16))
            vst = B(_sb(nc, "vst", [128, NT, 6], F32))
            stmp = [B(_sb(nc, "stmp%d" % s, [128, 128], F32)) for s in range(2)]
            tmpr = [B(_sb(nc, "tmpr%d" % s, [128, 512], F32)) for s in range(2)]
            pss = [B(_ps(nc, "psc%d" % s, [128, 512], F32)) for s in range(8)]
            psi = [0]

            def nps():
                k = psi[0] % 8
                psi[0] += 1
                return ("psc", k), pss[k]
            P.dma("pool", wU[:], rows_view(G.ab_w_in[0, :, 0:1024]), writes=["wU"], semkey="wU")
            P.dma("pool", wV[:], rows_view(G.ab_w_in[0, :, 1024:2048]), writes=["wV"], semkey="wV")
            P.dma("pool", woA[:], rows_view(G.ab_w_out[0, 0:1024, :]), writes=["woA"], semkey="woA")
            P.dma("sp", sgw, G.sgu_w[0].rearrange("g p q -> p g q"), writes=["normB"], semkey="sgw")
            for g in range(8):
                pk, ps = nps()
                P.tr(ps[:, 0:128], sgw[:, g, :], G.ident[:], reads=["normB", "ident"], writes=[pk])
                P.dve(lambda e, g=g, ps=ps: e.tensor_copy(out=sgwT[:, g, :], in_=ps[:, 0:128]),
                      reads=[pk], writes=["sgwT"])
            row = gv[0:1, :]
            for (src, dst, nm) in ((G.sgu_b[0:1].rearrange("o g p -> o (g p)"), biasB, "biasB"),
                                   (G.sgu_norm[0:1, :], normB, "normB")):
                P.dma("sp", row, src, writes=[("gv", 0), ("gv", 1)], semkey="sgrow")
                for hb in range(2):
                    pk, ps = nps()
                    P.mm(ps[:, :], [(G.ones[0:1, 0:128], row[0:1, hb * 512:(hb + 1) * 512])],
                         reads=["ones", ("gv", 0), ("gv", 1)], writes=[pk])
                    P.act(lambda e, dst=dst, hb=hb, ps=ps: e.activation(out=dst[:, hb * 512:(hb + 1) * 512],
                                                                         in_=ps[:, :], func=AF.Identity),
                          reads=[pk], writes=[nm])
            ui = 0
            for bi, (a, n) in enumerate(TOKBLK):
                for g in range(8):
                    pk, ps = nps()
                    P.mm(ps[:, 0:n], [(wU[:, k, g * 128:(g + 1) * 128], hT[:, k, a:a + n]) for k in range(8)],
                         reads=["wU", "hTall"], writes=[pk])
                    P.act(lambda e, g=g, ps=ps, n=n: e.activation(out=guT[:, g, 0:n], in_=ps[:, 0:n],
                                                                  func=AF.Gelu_apprx_tanh),
                          reads=[pk], writes=[("guT", g)])
                for tt in range(n // 128):
                    t = a // 128 + tt
                    s = t % 2
                    for hb in range(2):
                        pk, ps = nps()
                        P.mm(ps[:, :], [(hT[:, k, t * 128:(t + 1) * 128], wV[:, k, hb * 512:(hb + 1) * 512])
                                        for k in range(8)], reads=["wV", "hTall"], writes=[pk])
                        P.act(lambda e, hb=hb, ps=ps: e.activation(out=gv[:, hb * 512:(hb + 1) * 512], in_=ps[:, :],
                                                                   func=AF.Gelu_apprx_tanh),
                              reads=[pk], writes=[("gv", hb)])
                    P.act(lambda e, t=t: e.activation(out=sjunk[:], in_=gv[:], func=AF.Square,
                                                      accum_out=vst[:, t, 0:1]),
                          reads=[("gv", 0), ("gv", 1)], writes=["sjunk", ("vst", t)])
                    P.dve(lambda e, t=t: e.tensor_scalar(out=vst[:, t, 1:2], in0=vst[:, t, 0:1], scalar1=1.0 / D,
                                                         scalar2=EPS, op0=ALU.mult, op1=ALU.add),
                          reads=[("vst", t)], writes=[("vst1", t)])
                    P.act(lambda e, t=t: e.activation(out=vst[:, t, 2:3], in_=vst[:, t, 1:2], func=AF.Sqrt),
                          reads=[("vst1", t)], writes=[("vst2", t)])
                    P.dve(lambda e, t=t: e.reciprocal(out=vst[:, t, 3:4], in_=vst[:, t, 2:3]),
                          reads=[("vst2", t)], writes=[("vst3", t)])
                    P.dve(lambda e, t=t, s=s: e.scalar_tensor_tensor(out=vc[s][:], in0=gv[:], scalar=vst[:, t, 3:4],
                                                                     in1=normB[:], op0=ALU.mult, op1=ALU.mult),
                          reads=[("gv", 0), ("gv", 1), ("vst3", t), "normB"], writes=[("vc", s)])
                    for g in range(8):
                        pk, ps = nps()
                        P.mm(ps[:, 0:128], [(vc[s][:, g * 128:(g + 1) * 128], sgwT[:, g, :])],
                             reads=[("vc", s), "sgwT"], writes=[pk])
                        ss = g % 2
                        P.dve(lambda e, ss=ss, ps=ps, g=g: e.tensor_tensor(
                            out=stmp[ss][:], in0=ps[:, 0:128], in1=biasB[:, g * 128:(g + 1) * 128], op=ALU.add),
                            reads=[pk, "biasB"], writes=[("stmp", ss)])
                        P.dve(lambda e, ss=ss, g=g, s=s, tt=tt: e.tensor_tensor(
                            out=yaT[s][:, g, :], in0=stmp[ss][:], in1=guT[:, g, tt * 128:(tt + 1) * 128], op=ALU.mult),
                            reads=[("stmp", ss), ("guT", g)], writes=[("yaT", s, g)])
                    for db in range(2):
                        pk, ps = nps()
                        P.mm(ps[:, :], [(yaT[s][:, k, :], woA[:, k, db * 512:(db + 1) * 512]) for k in range(8)],
                             reads=[("yaT", s, k) for k in range(8)] + ["woA"], writes=[pk])
                        resid_update(P, G, t, ps[:, :], pk, G.g1b, db * 512, 512, tmpr[ui % 2], ("tmpr", ui % 2))
                        ui += 1
            P.barrier()
            P.flush()


def phase_moe(P, G, i, ntiles):
    nc = P.nc
    with ExitStack() as ph:
        A = ph.enter_context
        tT = A(_sb(nc, "tT", [128, 8, T_ALL], BF16))
        gates = A(_sb(nc, "gates", [128, NT, 32], F32))
        with ExitStack() as p1:
            B = p1.enter_context
            scr = make_scr(P, B, 4, with_t32=True)
            wr = B(_sb(nc, "wr", [128, 8, 36], F32))
            br = B(_sb(nc, "br", [1, 36], F32))
            rt = [B(_sb(nc, "rt%d" % s, [128, 256], F32)) for s in range(2)]
            psr = [B(_ps(nc, "psr%d" % s, [128, 512], F32)) for s in range(2)]
            P.dma("sp", wr[:, :, 0:4], rows_view(G.moe_w_grp[i]), writes=["wr"], semkey="wr")
            P.dma("sp", wr[:, :, 4:36], rows_view(G.moe_w_exp[i]), writes=["wr"], semkey="wr")
            P.dma("sp", br[0:1, 0:4], G.moe_b_grp[i:i + 1, :], writes=["br"], semkey="br")
            P.dma("sp", br[0:1, 4:36], G.moe_b_exp[i:i + 1, :], writes=["br"], semkey="br")

            def router(t, s):
                R = rt[s]
                rk = ("rt", s)
                pk = ("psr", s)
                pairs = [(scr.t32[s][:, c, :], wr[:, c, :]) for c in range(8)]
                pairs.append((G.ones[0:1, 0:128], br[0:1, :]))
                P.mm(psr[s][:, 0:36], pairs, reads=[("t32", s, c) for c in range(8)] + ["wr", "br", "ones"],
                     writes=[pk])
                def d(fn, nm):
                    P.dve(fn, reads=[rk, pk], writes=[rk])
                d(lambda e: e.tensor_copy(out=R[:, 0:36], in_=psr[s][:, 0:36]), "lg")
                d(lambda e: e.reduce_max(out=R[:, 40:41], in_=R[:, 0:4], axis=mybir.AxisListType.X), "gmax")
                d(lambda e: e.tensor_scalar(out=R[:, 41:42], in0=R[:, 40:41], scalar1=-1.0, scalar2=None,
                                            op0=ALU.mult), "ngmax")
                P.act(lambda e: e.activation(out=R[:, 36:40], in_=R[:, 0:4], func=AF.Exp, bias=R[:, 41:42],
                                             accum_out=R[:, 42:43]), reads=[rk], writes=[rk])
                d(lambda e: e.reciprocal(out=R[:, 43:44], in_=R[:, 42:43]), "ptop")
                d(lambda e: e.tensor_scalar(out=R[:, 44:48], in0=R[:, 0:4], scalar1=R[:, 40:41], scalar2=None,
                                            op0=ALU.is_equal), "gmask")
                d(lambda e: e.tensor_scalar(out=R[:, 48:52], in0=R[:, 44:48], scalar1=BIG, scalar2=-BIG,
                                            op0=ALU.mult, op1=ALU.add), "pen")
                for g in range(4):
                    d(lambda e, g=g: e.tensor_scalar(out=R[:, 64 + 8 * g:72 + 8 * g], in0=R[:, 4 + 8 * g:12 + 8 * g],
                                                     scalar1=R[:, 48 + g:49 + g], scalar2=None, op0=ALU.add), "ml")
                d(lambda e: e.reduce_max(out=R[:, 52:53], in_=R[:, 64:96], axis=mybir.AxisListType.X), "m1")
                d(lambda e: e.tensor_scalar(out=R[:, 96:128], in0=R[:, 64:96], scalar1=R[:, 52:53], scalar2=None,
                                            op0=ALU.is_equal), "mask1")
                d(lambda e: e.scalar_tensor_tensor(out=R[:, 128:160], in0=R[:, 96:128], scalar=-BIG, in1=R[:, 64:96],
                                                   op0=ALU.mult, op1=ALU.add), "ml2")
                d(lambda e: e.reduce_max(out=R[:, 53:54], in_=R[:, 128:160], axis=mybir.AxisListType.X), "m2")
                d(lambda e: e.tensor_scalar(out=R[:, 160:192], in0=R[:, 128:160], scalar1=R[:, 53:54], scalar2=None,
                                            op0=ALU.is_equal), "mask2")
                d(lambda e: e.tensor_tensor(out=R[:, 54:55], in0=R[:, 53:54], in1=R[:, 52:53], op=ALU.subtract), "d")
                P.act(lambda e: e.activation(out=R[:, 55:56], in_=R[:, 54:55], func=AF.Exp), reads=[rk], writes=[rk])
                d(lambda e: e.tensor_scalar(out=R[:, 56:57], in0=R[:, 55:56], scalar1=1.0, scalar2=None,
                                            op0=ALU.add), "den")
                d(lambda e: e.reciprocal(out=R[:, 57:58], in_=R[:, 56:57]), "rden")
                d(lambda e: e.tensor_tensor(out=R[:, 58:59], in0=R[:, 57:58], in1=R[:, 43:44], op=ALU.mult), "w1")
                d(lambda e: e.tensor_tensor(out=R[:, 59:60], in0=R[:, 58:59], in1=R[:, 55:56], op=ALU.mult), "w2")
                d(lambda e: e.tensor_scalar(out=R[:, 192:224], in0=R[:, 96:128], scalar1=R[:, 58:59], scalar2=None,
                                            op0=ALU.mult), "t1")
                P.dve(lambda e: e.scalar_tensor_tensor(out=gates[:, t, :], in0=R[:, 160:192], scalar=R[:, 59:60],
                                                       in1=R[:, 192:224], op0=ALU.mult, op1=ALU.add),
                      reads=[rk], writes=["gates"])
            modulate(P, G, tT, 3, 4, ntiles, scr, router=router)
            P.barrier()
            P.flush()
        with ExitStack() as p2:
            B = p2.enter_context
            wg = [B(_sb(nc, "wg%d" % s, [128, 8, 512], BF16)) for s in range(2)]
            wu = [B(_sb(nc, "wu%d" % s, [128, 8, 512], BF16)) for s in range(2)]
            wd = [B(_sb(nc, "wd%d" % s, [128, 4, 1024], BF16)) for s in range(2)]
            he = [B(_sb(nc, "he%d" % s, [128, 4, 512], BF16)) for s in range(2)]
            sg = [B(_sb(nc, "sg%d" % s, [128, 512], F32)) for s in range(2)]
            tmpr = [B(_sb(nc, "tmpe%d" % s, [128, 512], F32)) for s in range(2)]
            psg = [B(_ps(nc, "psg%d" % s, [128, 512], F32)) for s in range(2)]
            psu = [B(_ps(nc, "psu%d" % s, [128, 512], F32)) for s in range(2)]
            psy = [B(_ps(nc, "psy%d" % s, [128, 512], F32)) for s in range(4)]
            blks = [b for b in TOKBLK if b[0] < ntiles * 128]
            steps = [(ex, bi) for ex in range(NEXP) for bi in range(len(blks))]
            cnt = {"ci": 0, "yi": 0}

            def emit_gu(i):
                ex, bi = steps[i]
                s = ex % 2
                if bi == 0:
                    P.dma("pool", wg[s][:], rows_view(G.moe_w_gate[i_layer, ex]), writes=[("wg", s)], semkey="wg%d" % s)
                    P.dma("pool", wu[s][:], rows_view(G.moe_w_up[i_layer, ex]), writes=[("wu", s)], semkey="wu%d" % s)
                    P.dma("pool", wd[s][:], rows_view(G.moe_w_down[i_layer, ex]), writes=[("wd", s)], semkey="wd%d" % s)
                (a, n) = blks[bi]
                hs = i % 2
                for m in range(4):
                    cs = cnt["ci"] % 2
                    cnt["ci"] += 1
                    P.mm(psg[cs][:, 0:n], [(wg[s][:, k, m * 128:(m + 1) * 128], tT[:, k, a:a + n]) for k in range(8)],
                         reads=[("wg", s), "tT"], writes=[("psg", cs)])
                    P.mm(psu[cs][:, 0:n], [(wu[s][:, k, m * 128:(m + 1) * 128], tT[:, k, a:a + n]) for k in range(8)],
                         reads=[("wu", s), "tT"], writes=[("psu", cs)])
                    P.act(lambda e, cs=cs, n=n: e.activation(out=sg[cs][:, 0:n], in_=psg[cs][:, 0:n], func=AF.Silu),
                          reads=[("psg", cs)], writes=[("sg", cs)])
                    P.dve(lambda e, cs=cs, n=n, hs=hs, m=m: e.tensor_tensor(
                        out=he[hs][:, m, 0:n], in0=sg[cs][:, 0:n], in1=psu[cs][:, 0:n], op=ALU.mult),
                        reads=[("sg", cs), ("psu", cs)], writes=[("he", hs, m)])

            def emit_down(i):
                ex, bi = steps[i]
                s = ex % 2
                (a, n) = blks[bi]
                hs = i % 2
                for tt in range(n // 128):
                    t = a // 128 + tt
                    for db in range(2):
                        ys = cnt["yi"] % 4
                        cnt["yi"] += 1
                        yi = cnt["yi"]
                        P.mm(psy[ys][:, :], [(he[hs][:, m, tt * 128:(tt + 1) * 128], wd[s][:, m, db * 512:(db + 1) * 512])
                                             for m in range(4)],
                             reads=[("he", hs, m) for m in range(4)] + [("wd", s)], writes=[("psy", ys)])
                        resid_update(P, G, t, psy[ys][:, :], ("psy", ys), G.g2b, db * 512, 512, tmpr[yi % 2],
                                     ("tmpe", yi % 2), gate=gates[:, t, ex:ex + 1])

            i_layer = i
            emit_gu(0)
            for si in range(len(steps)):
                if si + 1 < len(steps):
                    emit_gu(si + 1)
                emit_down(si)
            P.barrier()
            P.flush()


def phase_out(P, G):
    P.dma("sp", G.out.rearrange("(t p) d -> p t d", p=128), G.xl[:, 0:NT_L, :],
          reads=[("xl", t) for t in range(NT_L)], semkey="st")
    P.barrier()
    P.flush()


WEIGHT_NAMES = ["c_ctx", "ada_w", "ada_b", "ab_w_in", "sgu_norm", "sgu_w", "sgu_b", "conv_w", "ab_w_out",
                "cd_w_in", "s5_a_re", "s5_a_im", "s5_log_dt", "s5_b_re", "s5_b_im", "s5_c_re", "s5_c_im", "s5_d",
                "s5_w_glu", "mla_q_norm", "mla_kv_norm", "mla_w_uq", "mla_w_uk", "mla_w_uv", "mla_qn_gain",
                "mla_kn_gain", "cd_w_out", "moe_w_grp", "moe_b_grp", "moe_w_exp", "moe_b_exp", "moe_w_gate",
                "moe_w_up", "moe_w_down"]


def build(shapes, stages):
    nc = bass.Bass("TRN2", target_bir_lowering=False)
    G = SimpleNamespace()
    G.x = nc.dram_tensor("x", [SEQ, D], F32, kind="ExternalInput").ap()
    G.ctx = nc.dram_tensor("ctx", [CTX, D], F32, kind="ExternalInput").ap()
    G.c = nc.dram_tensor("c", [1, D], F32, kind="ExternalInput").ap()
    used = set(shapes.keys())
    for nm in WEIGHT_NAMES:
        if nm in used:
            shp = list(shapes[nm])
            if nm == "c_ctx":
                shp = [1, D]
            setattr(G, nm, nc.dram_tensor(nm, shp, F32, kind="ExternalInput").ap())
    G.out = nc.dram_tensor("out", [SEQ, D], F32, kind="ExternalOutput").ap()
    G.cd_parts = CD_PARTS
    if "mix1" in stages:
        G.ropC = nc.dram_tensor("ropC", [SEQ, 32], F32, kind="ExternalInput").ap()
        G.ropS = nc.dram_tensor("ropS", [SEQ, 32], F32, kind="ExternalInput").ap()
        G.zs = nc.dram_tensor("zs_scr", [12, 128, T_ALL], BF16, kind="Internal").ap()
        G.mixs = nc.dram_tensor("mix_scr", [8, 128, SEQ], BF16, kind="Internal").ap()
        G.qs = nc.dram_tensor("q_scr", [8, 96, SEQ], BF16, kind="Internal").ap()
        G.ks = nc.dram_tensor("k_scr", [8, 96, T_ALL], BF16, kind="Internal").ap()
        G.vs = nc.dram_tensor("v_scr", [NT, 128, 8, 65], BF16, kind="Internal").ap()
    with ExitStack() as st:
        E = st.enter_context
        P = Prog(nc, E)
        G.xl = E(_sb(nc, "xl", [128, NT, D], F32))
        G.g1b = E(_sb(nc, "gtile", [128, 2, D], F32))
        G.g2b = G.g1b
        G.modT = E(_sb(nc, "modT", [128, 6, 8, 2], F32))
        G.ident = E(_sb(nc, "ident", [128, 128], F32))
        G.iot = E(_sb(nc, "iot", [128, 128], I32))
        G.ones = E(_sb(nc, "ones", [128, 128], F32))
        G.onesb = E(_sb(nc, "onesb", [128, 128], BF16))
        phase_init(P, G)
        for stg in stages:
            if stg == "ada0":
                phase_ada(P, G, 0, [0, 1, 2, 3, 6, 7, 8, 9, 4, 5])
            elif stg == "mix0":
                phase_mixer_ab(P, G)
            elif stg == "moe0":
                phase_ada(P, G, 0, [10, 11])
                phase_moe(P, G, 0, NT)
            elif stg == "ada1":
                phase_ada(P, G, 1, [0, 1, 2, 3, 6, 7, 8, 9, 4, 5])
            elif stg == "mix1":
                phase_mixer_cd(P, G)
            elif stg == "moe1":
                phase_ada(P, G, 1, [10, 11])
                phase_moe(P, G, 1, NT_L)
        phase_out(P, G)
    return nc


ALL_STAGES = ["ada0", "mix0", "moe0", "ada1", "mix1", "moe1"]
CD_PARTS = ("s5", "mla")


def run(inputs, stages):
    needed = set(["c_ctx", "ada_w", "ada_b"])
    if "mix0" in stages:
        needed |= {"ab_w_in", "sgu_norm", "sgu_w", "sgu_b", "conv_w", "ab_w_out"}
    if "mix1" in stages:
        needed |= {"cd_w_in", "s5_a_re", "s5_a_im", "s5_log_dt", "s5_b_re", "s5_b_im", "s5_c_re", "s5_c_im", "s5_d",
                   "s5_w_glu", "mla_q_norm", "mla_kv_norm", "mla_w_uq", "mla_w_uk", "mla_w_uv", "mla_qn_gain",
                   "mla_kn_gain", "cd_w_out"}
    if "moe0" in stages or "moe1" in stages:
        needed |= {"moe_w_grp", "moe_b_grp", "moe_w_exp", "moe_b_exp", "moe_w_gate", "moe_w_up", "moe_w_down"}
    shapes = {k: inputs[k].shape for k in needed}
    nc = build(shapes, stages)
    shared = {}
    for k in needed:
        a = np.ascontiguousarray(np.asarray(inputs[k], dtype=np.float32))
        if k == "c_ctx":
            a = a.reshape(1, D)
        shared[k] = a
    if "mix1" in stages:
        pos = np.arange(SEQ)
        row = (pos // 64).astype(np.float32)
        colp = (pos % 64).astype(np.float32)
        inv = (np.float32(10000.0) ** (-np.arange(8, dtype=np.float32) / np.float32(8))).astype(np.float32)
        ar = (row[:, None] * inv[None, :]).astype(np.float32)
        ac = (colp[:, None] * inv[None, :]).astype(np.float32)
        cr, sr, cc, sc_ = np.cos(ar), np.sin(ar), np.cos(ac), np.sin(ac)
        shared["ropC"] = np.ascontiguousarray(np.concatenate([cr, cr, cc, cc], axis=1).astype(np.float32))
        shared["ropS"] = np.ascontiguousarray(np.concatenate([-sr, sr, -sc_, sc_], axis=1).astype(np.float32))
    x = np.asarray(inputs["x"], dtype=np.float32)
    ctx = np.asarray(inputs["ctx"], dtype=np.float32)
    c = np.asarray(inputs["c"], dtype=np.float32)
    in_maps = []
    for b in range(8):
        m = dict(shared)
        m["x"] = np.ascontiguousarray(x[b])
        m["ctx"] = np.ascontiguousarray(ctx[b])
        m["c"] = np.ascontiguousarray(c[b].reshape(1, D))
        in_maps.append(m)
    res = run_bass_kernel_spmd(nc, in_maps, core_ids=list(range(8)))
    return np.stack([np.asarray(r["out"], dtype=np.float32) for r in res.results], axis=0)


def kernel(**inputs):
    return run(inputs, ALL_STAGES)


def _rot(lst, name):
    st = [0]

    def f():
        k = st[0] % len(lst)
        st[0] += 1
        return (name, k), lst[k]
    return f


S5OPT = {"pool_rr": True, "pool_rotin": False, "prefetch": True}
TWO_PI = 2.0 * math.pi
PI_SAFE = 3.1415925


def Prog_mm1(P, out, lhsT, rhs, start, stop, reads=(), writes=()):
    P.op("pe", lambda e: e.matmul(out, lhsT, rhs, start=start, stop=stop), reads, writes)


def phase_mixer_cd(P, G):
    nc = P.nc
    X = mybir.AxisListType.X
    with ExitStack() as ph:
        A = ph.enter_context
        mst = A(_sb(nc, "mst", [128, NT, 40], F32))
        with ExitStack() as p1:
            B = p1.enter_context
            hT = B(_sb(nc, "hT1", [128, 8, T_ALL], BF16))
            with ExitStack() as p0:
                scr = make_scr(P, p0.enter_context, 4)
                modulate(P, G, hT, 0, 1, NT, scr)
                P.barrier()
                P.flush()
            wI = B(_sb(nc, "wI", [128, 8, 1568], BF16))
            stg = [B(_sb(nc, "zst%d" % s, [128, 512], BF16)) for s in range(4)]
            sqj = B(_sb(nc, "sqj", [128, 512], BF16))
            nps = _rot([B(_ps(nc, "psd%d" % s, [128, 512], F32)) for s in range(8)], "psd")
            P.dma("pool", wI[:], rows_view(G.cd_w_in[0]), writes=["wI"], semkey="wI")
            si = 0
            for j in range(12):
                for (a, n) in TOKBLK:
                    pk, ps = nps()
                    P.mm(ps[:, 0:n], [(wI[:, k, j * 128:(j + 1) * 128], hT[:, k, a:a + n]) for k in range(8)],
                         reads=["wI"], writes=[pk])
                    s = si % 4
                    si += 1
                    if si % 2:
                        P.act(lambda e, s=s, ps=ps, n=n: e.activation(out=stg[s][:, 0:n], in_=ps[:, 0:n], func=AF.Identity),
                              reads=[pk], writes=[("zst", s)])
                    else:
                        P.dve(lambda e, s=s, ps=ps, n=n: e.tensor_copy(out=stg[s][:, 0:n], in_=ps[:, 0:n]),
                              reads=[pk], writes=[("zst", s)])
                    P.dma("sp", G.zs[j, :, a:a + n], stg[s][:, 0:n], reads=[("zst", s)], writes=[("zs", j)],
                          semkey="zst%d" % s)
            for t in range(NT):
                hs = [hT[:, k, t * 128:(t + 1) * 128] for k in range(8)]
                pk, ps = nps()
                P.mm(ps[:, :], [(hs[k], wI[:, k, 512:1024]) for k in range(8)], reads=["wI"], writes=[pk])
                P.act(lambda e, ps=ps, t=t: e.activation(out=sqj[:, :], in_=ps[:, :], func=AF.Square,
                                                         accum_out=mst[:, t, 0:1]), reads=[pk], writes=["sqj", "mst"])
                pk, ps = nps()
                P.mm(ps[:, :], [(hs[k], wI[:, k, 1024:1536]) for k in range(8)], reads=["wI"], writes=[pk])
                P.act(lambda e, ps=ps, t=t: e.activation(out=sqj[:, 0:256], in_=ps[:, 0:256], func=AF.Square,
                                                         accum_out=mst[:, t, 1:2]), reads=[pk], writes=["sqj", "mst"])
                P.act(lambda e, ps=ps, t=t: e.activation(out=sqj[:, 256:512], in_=ps[:, 256:512], func=AF.Square,
                                                         accum_out=mst[:, t, 2:3]), reads=[pk], writes=["sqj", "mst"])
                pk, ps = nps()
                P.mm(ps[:, 0:32], [(hs[k], wI[:, k, 1536:1568]) for k in range(8)], reads=["wI"], writes=[pk])
                P.dve(lambda e, ps=ps, t=t: e.tensor_copy(out=mst[:, t, 8:40], in_=ps[:, 0:32]),
                      reads=[pk], writes=["mst"])
            m = ["mst"]
            P.dve(lambda e: e.tensor_tensor(out=mst[:, :, 5], in0=mst[:, :, 0], in1=mst[:, :, 1], op=ALU.add), m, m)
            P.dve(lambda e: e.tensor_scalar(out=mst[:, :, 5], in0=mst[:, :, 5], scalar1=1.0 / 768, scalar2=EPS,
                                            op0=ALU.mult, op1=ALU.add), m, m)
            P.act(lambda e: e.activation(out=mst[:, :, 6], in_=mst[:, :, 5], func=AF.Sqrt), m, m)
            P.dve(lambda e: e.reciprocal(out=mst[:, :, 3], in_=mst[:, :, 6]), m, m)
            P.dve(lambda e: e.tensor_scalar(out=mst[:, :, 5], in0=mst[:, :, 2], scalar1=1.0 / 256, scalar2=EPS,
                                            op0=ALU.mult, op1=ALU.add), m, m)
            P.act(lambda e: e.activation(out=mst[:, :, 6], in_=mst[:, :, 5], func=AF.Sqrt), m, m)
            P.dve(lambda e: e.reciprocal(out=mst[:, :, 4], in_=mst[:, :, 6]), m, m)
            P.barrier()
            P.flush()
        if "s5" in G.cd_parts:
            _cd_s5(P, G, nc)
        if "mla" in G.cd_parts:
            _cd_mla(P, G, nc, mst)
        with ExitStack() as p4:
            B = p4.enter_context
            mixT = B(_sb(nc, "mixT", [128, 8, SEQ], BF16))
            woC = B(_sb(nc, "woC", [128, 8, 1024], BF16))
            tmpr = [B(_sb(nc, "tmpo%d" % s, [128, 512], F32)) for s in range(2)]
            nps = _rot([B(_ps(nc, "pso%d" % s, [128, 512], F32)) for s in range(4)], "pso")
            ks = []
            if "s5" in G.cd_parts:
                ks += [0, 1, 2, 3]
            if "mla" in G.cd_parts:
                ks += [4, 5, 6, 7]
            for j in ks:
                P.dma("sp", mixT[:, j, :], G.mixs[j], writes=[("mixT", j)], semkey="mixT%d" % j)
            P.dma("pool", woC[:], rows_view(G.cd_w_out[0]), writes=["woC"], semkey="woC")
            ui = 0
            for t in range(NT_L):
                for db in range(2):
                    pk, ps = nps()
                    P.mm(ps[:, :], [(mixT[:, k, t * 128:(t + 1) * 128], woC[:, k, db * 512:(db + 1) * 512]) for k in ks],
                         reads=[("mixT", k) for k in ks] + ["woC"], writes=[pk])
                    resid_update(P, G, t, ps[:, :], pk, G.g1b, db * 512, 512, tmpr[ui % 2], ("tmpo", ui % 2))
                    ui += 1
            P.barrier()
            P.flush()


def _cd_s5(P, G, nc):
    T = T_ALL
    with ExitStack() as p2:
        Bp = p2.enter_context
        V = Bp(_sb(nc, "s5v", [128, 24, 32], F32))
        VI = Bp(_sb(nc, "s5vi", [128, 32], I32))
        hpi = Bp(_sb(nc, "hpi", [128, 1], F32))
        dsk = Bp(_sb(nc, "dsk", [128, 4], F32))
        ldrow = Bp(_sb(nc, "ldrow", [1, 64], F32))
        gT = Bp(_sb(nc, "s5g", [128, 4, SEQ], BF16))
        AR, AI, LDT, DT, TMP, MAG, TH, THR, SN, AB, CS, ABR, ABI, DEN, RDEN, NR, FRE, FIM, NFIM, T2, T3 = range(21)
        with ExitStack() as pa:
            B = pa.enter_context
            NF = B(_sb(nc, "s5nf", [128, T], F32))
            TSb = [B(_sb(nc, "s5ts%d" % s, [128, T], F32)) for s in range(2)]
            TCb = [B(_sb(nc, "s5tc%d" % s, [128, T], F32)) for s in range(2)]
            GR = B(_sb(nc, "s5gr", [128, T], F32))
            GI = B(_sb(nc, "s5gi", [128, T], F32))
            KI = GI[:].bitcast(I32)
            HRb = [B(_sb(nc, "s5hr%d" % s, [128, 512], BF16)) for s in range(2)]
            HIb = [B(_sb(nc, "s5hi%d" % s, [128, 512], BF16)) for s in range(2)]
            tmp = [B(_sb(nc, "s5tmp%d" % s, [128, 512], F32)) for s in range(4)]
            uTc = B(_sb(nc, "s5u", [128, T], BF16))
            yacc = B(_sb(nc, "s5y", [128, SEQ], F32))
            XRE = [B(_sb(nc, "xre%d" % s, [128, 128], F32)) for s in range(2)]
            XIM = [B(_sb(nc, "xim%d" % s, [128, 128], F32)) for s in range(2)]
            BT = [B(_sb(nc, "bt%d" % s, [128, 128], F32)) for s in range(2)]
            BB = [B(_sb(nc, "bb%d" % s, [128, 2, 128], F32)) for s in range(2)]
            LB = [B(_sb(nc, "lb%d" % s, [128, 2, 128], BF16)) for s in range(2)]
            LC = [B(_sb(nc, "lc%d" % s, [128, 2, 128], BF16)) for s in range(2)]
            YY = [B(_sb(nc, "yy%d" % s, [32, 2, 128], F32)) for s in range(2)]
            nps = _rot([B(_ps(nc, "pss%d" % s, [128, 512], F32)) for s in range(8)], "pss")
            v = ["s5v"]
            for (src, slot) in ((G.s5_a_re, 0), (G.s5_a_im, 1)):
                for g2 in range(2):
                    P.dma("sp", V[g2 * 64:(g2 + 1) * 64, slot, :].rearrange("p (d q) -> p d q", d=2),
                          src[0].rearrange("d (q g) p -> g p d q", g=2)[g2], writes=v, semkey="s5v",
                          allow_slow_non_contiguous=True)
            P.dma("sp", ldrow[0:1, :], G.s5_log_dt[0:1].rearrange("o d g -> o (d g)"), writes=["ldrow"], semkey="ldrow")
            P.dma("sp", dsk[:, :], G.s5_d[0].rearrange("(k p) -> p k", p=128), writes=["dsk"], semkey="dsk",
                  allow_slow_non_contiguous=True)
            P.pool(lambda e: e.memset(hpi[:], math.pi / 2), writes=["hpi"])
            P.pool(lambda e: e.iota(NF[:], [[1, T]], base=1, channel_multiplier=0, allow_small_or_imprecise_dtypes=True),
                   writes=["NF"])
            pk, ps = nps()
            P.mm(ps[:, 0:64], [(G.ones[0:1, 0:128], ldrow[0:1, :])], reads=["ldrow"], writes=[pk])
            psv = ps[:, 0:64].rearrange("p (d q g) -> p d q g", d=2, g=2)
            P.dve(lambda e: e.tensor_copy(out=V[0:64, 2, :].rearrange("p (d q) -> p d q", d=2), in_=psv[0:64, :, :, 0]),
                  reads=[pk], writes=v)
            P.dve(lambda e: e.tensor_copy(out=V[64:128, 2, :].rearrange("p (d q) -> p d q", d=2), in_=psv[64:128, :, :, 1]),
                  reads=[pk], writes=v)

            def vv(i):
                return V[:, i, :]

            def tt(o, a, b, op):
                P.dve(lambda e: e.tensor_tensor(out=vv(o), in0=vv(a), in1=vv(b), op=op), v, v)
            P.act(lambda e: e.activation(out=vv(DT), in_=vv(LDT), func=AF.Exp), v, v)
            tt(TMP, DT, AR, ALU.mult)
            P.act(lambda e: e.activation(out=vv(MAG), in_=vv(TMP), func=AF.Exp), v, v)
            tt(TH, DT, AI, ALU.mult)
            P.dve(lambda e: e.tensor_scalar(out=VI[:, :], in0=vv(TH), scalar1=1.0 / TWO_PI, scalar2=None, op0=ALU.mult), v, v)
            P.dve(lambda e: e.scalar_tensor_tensor(out=vv(THR), in0=VI[:, :], scalar=-TWO_PI, in1=vv(TH), op0=ALU.mult,
                                                   op1=ALU.add), v, v)
            P.dve(lambda e: e.tensor_scalar(out=vv(THR), in0=vv(THR), scalar1=-PI_SAFE, scalar2=PI_SAFE, op0=ALU.max,
                                            op1=ALU.min), v, v)
            P.act(lambda e: e.activation(out=vv(SN), in_=vv(THR), func=AF.Sin), v, v)
            P.act(lambda e: e.activation(out=vv(AB), in_=vv(THR), func=AF.Abs), v, v)
            P.act(lambda e: e.activation(out=vv(CS), in_=vv(AB), func=AF.Sin, scale=-1.0, bias=hpi[:, 0:1]),
                  v + ["hpi"], v)
            tt(ABR, MAG, CS, ALU.mult)
            tt(ABI, MAG, SN, ALU.mult)
            tt(DEN, AR, AR, ALU.mult)
            tt(T2, AI, AI, ALU.mult)
            tt(DEN, DEN, T2, ALU.add)
            P.dve(lambda e: e.reciprocal(out=vv(RDEN), in_=vv(DEN)), v, v)
            P.dve(lambda e: e.tensor_scalar(out=vv(NR), in0=vv(ABR), scalar1=-1.0, scalar2=None, op0=ALU.add), v, v)
            tt(T2, NR, AR, ALU.mult)
            tt(T3, ABI, AI, ALU.mult)
            tt(T2, T2, T3, ALU.add)
            tt(FRE, T2, RDEN, ALU.mult)
            tt(T2, ABI, AR, ALU.mult)
            tt(T3, NR, AI, ALU.mult)
            tt(T2, T2, T3, ALU.subtract)
            tt(FIM, T2, RDEN, ALU.mult)
            P.dve(lambda e: e.tensor_scalar(out=vv(NFIM), in0=vv(FIM), scalar1=-1.0, scalar2=None, op0=ALU.mult), v, v)
            P.barrier()
            vr = []

            iters = [(ch, pic, d) for ch in range(4) for pic in range(4) for d in range(2)]

            def prep(it):
                ch, pic, d = iters[it]
                s = it % 2
                q = ch * 4 + pic
                col = d * 16 + q
                c0 = 32 * pic
                xk, bk, lbk, lck, yk = ("X", s), ("BB", s), ("LB", s), ("LC", s), ("YY", s)
                TS, TC = TSb[s], TCb[s]
                tsk, tck = ("TS", s), ("TC", s)
                P.pool(lambda e: e.memset(XRE[s][:], 0.0), writes=[xk])
                P.pool(lambda e: e.memset(XIM[s][:], 0.0), writes=[xk])
                P.pool(lambda e: e.memset(YY[s][:], 0.0), writes=[yk])
                P.pool(lambda e: e.memset(LC[s][:], 0.0), writes=[lck])
                for g2 in range(2):
                    g = 2 * q + g2
                    cc = c0 + 16 * g2
                    P.dma("sp", XRE[s][g2 * 64:(g2 + 1) * 64, cc:cc + 16], G.s5_b_re[0, d, g], writes=[xk],
                          semkey="X%d" % s)
                    P.dma("sp", XIM[s][g2 * 64:(g2 + 1) * 64, cc:cc + 16], G.s5_b_im[0, d, g], writes=[xk],
                          semkey="X%d" % s)
                    P.dma("sp", YY[s][g2 * 16:(g2 + 1) * 16, 0, g2 * 64:(g2 + 1) * 64], G.s5_c_re[0, d, g],
                          writes=[yk], semkey="Y%d" % s)
                    P.dma("sp", YY[s][g2 * 16:(g2 + 1) * 16, 1, g2 * 64:(g2 + 1) * 64], G.s5_c_im[0, d, g],
                          writes=[yk], semkey="Y%d" % s)
                fre, fim, nfim = V[:, FRE, col:col + 1], V[:, FIM, col:col + 1], V[:, NFIM, col:col + 1]
                P.dve(lambda e: e.tensor_scalar(out=BT[s][:], in0=XRE[s][:], scalar1=fre, scalar2=None, op0=ALU.mult),
                      reads=[xk], writes=[("BT", s)])
                P.dve(lambda e: e.scalar_tensor_tensor(out=BB[s][:, 0, :], in0=XIM[s][:], scalar=nfim, in1=BT[s][:],
                                                       op0=ALU.mult, op1=ALU.add), reads=[xk, ("BT", s)], writes=[bk])
                P.dve(lambda e: e.tensor_scalar(out=BT[s][:], in0=XIM[s][:], scalar1=fre, scalar2=None, op0=ALU.mult),
                      reads=[xk], writes=[("BT", s)])
                P.dve(lambda e: e.scalar_tensor_tensor(out=BB[s][:, 1, :], in0=XRE[s][:], scalar=fim, in1=BT[s][:],
                                                       op0=ALU.mult, op1=ALU.add), reads=[xk, ("BT", s)], writes=[bk])
                for ri in range(2):
                    pk, ps = nps()
                    P.tr(ps[:, 0:128], BB[s][:, ri, :], G.ident[:], reads=[bk], writes=[pk])
                    P.act(lambda e, ri=ri, ps=ps: e.activation(out=LB[s][:, ri, :], in_=ps[:, 0:128], func=AF.Identity),
                          reads=[pk], writes=[lbk])
                    pk, ps = nps()
                    P.tr(ps[:, 0:32], YY[s][0:32, ri, :], G.ident[0:32, 0:32], reads=[yk], writes=[pk])
                    P.act(lambda e, ri=ri, ps=ps: e.activation(out=LC[s][:, ri, c0:c0 + 32], in_=ps[:, 0:32],
                                                                func=AF.Identity, scale=(1.0 if ri == 0 else -1.0)),
                          reads=[pk], writes=[lck])
                thr = V[:, THR, col:col + 1]
                P.act(lambda e: e.activation(out=TS[:, :], in_=NF[:, :], func=AF.Identity, scale=thr),
                      reads=["NF"], writes=[tsk])
                KT = TC[:].bitcast(I32)
                rr = P.pool if S5OPT["pool_rr"] else P.dve
                rr(lambda e: e.tensor_scalar(out=KT, in0=TS[:, :], scalar1=1.0 / TWO_PI, scalar2=0.0, op0=ALU.mult,
                                             op1=ALU.add), reads=[tsk], writes=[tck])
                P.dve(lambda e: e.scalar_tensor_tensor(out=TS[:, :], in0=KT, scalar=-TWO_PI, in1=TS[:, :], op0=ALU.mult,
                                                       op1=ALU.add), reads=[tck, tsk], writes=[tsk])
                rr(lambda e: e.tensor_scalar(out=TS[:, :], in0=TS[:, :], scalar1=PI_SAFE, scalar2=-PI_SAFE,
                                             op0=ALU.min, op1=ALU.max), reads=[tsk], writes=[tsk])
                P.act(lambda e: e.activation(out=TC[:, :], in_=TS[:, :], func=AF.Abs), reads=[tsk, tck], writes=[tck])
                P.act(lambda e: e.activation(out=TS[:, :], in_=TS[:, :], func=AF.Sin), reads=[tsk, tck], writes=[tsk])
                P.act(lambda e: e.activation(out=TC[:, :], in_=TC[:, :], func=AF.Sin, scale=-1.0, bias=hpi[:, 0:1]),
                      reads=[tck, "hpi"], writes=[tck])

            if S5OPT["prefetch"]:
                prep(0)
            hb_i = 0
            for it, (ch, pic, d) in enumerate(iters):
                if not S5OPT["prefetch"]:
                    prep(it)
                s = it % 2
                q = ch * 4 + pic
                col = d * 16 + q
                lbk, lck = ("LB", s), ("LC", s)
                TS, TC = TSb[s], TCb[s]
                tsk, tck = ("TS", s), ("TC", s)
                if pic == 0 and d == 0:
                    P.dma("sp", uTc[:, :], G.zs[ch], writes=["uTc"], semkey="uTc")
                    P.dve(lambda e, ch=ch: e.tensor_scalar(out=yacc[:, :], in0=uTc[:, 0:SEQ], scalar1=dsk[:, ch:ch + 1],
                                                           scalar2=None, op0=ALU.mult),
                          reads=["uTc", "dsk"], writes=["yacc"])

                def tsl(tbl, b0, n, d=d):
                    if d == 0:
                        return tbl[:, b0:b0 + n]
                    return tbl[:, T - b0 - n:T - b0][:, ::-1]
                for bi, (a, n) in enumerate(TOKBLK):
                    if d == 0:
                        b0 = a + CTX if a < SEQ else a - SEQ
                    else:
                        b0 = a
                    pkr, psr = nps()
                    P.mm(psr[:, 0:n], [(LB[s][:, 0, :], uTc[:, a:a + n])], reads=[lbk, "uTc"], writes=[pkr])
                    pki, psi = nps()
                    P.mm(psi[:, 0:n], [(LB[s][:, 1, :], uTc[:, a:a + n])], reads=[lbk, "uTc"], writes=[pki])
                    tc_, ts_ = tsl(TC, b0, n), tsl(TS, b0, n)
                    ta, tb, tc2, td2 = tmp[0], tmp[1], tmp[2], tmp[3]
                    ka, kb, kc2, kd2 = ("tmp", 0), ("tmp", 1), ("tmp", 2), ("tmp", 3)
                    gr, gi = GR[:, b0:b0 + n], GI[:, b0:b0 + n]
                    P.act(lambda e, ta=ta, psr=psr, n=n: e.activation(out=ta[:, 0:n], in_=psr[:, 0:n], func=AF.Identity),
                          reads=[pkr], writes=[ka])
                    P.act(lambda e, tb=tb, psi=psi, n=n: e.activation(out=tb[:, 0:n], in_=psi[:, 0:n], func=AF.Identity),
                          reads=[pki], writes=[kb])
                    ri_ = P.pool if S5OPT["pool_rotin"] else P.dve
                    ri_(lambda e, ta=ta, tc2=tc2, ts_=ts_, n=n: e.tensor_tensor(out=tc2[:, 0:n], in0=ta[:, 0:n], in1=ts_,
                                                                                 op=ALU.mult),
                        reads=[ka, tsk], writes=[kc2])
                    ri_(lambda e, tb=tb, td2=td2, ts_=ts_, n=n: e.tensor_tensor(out=td2[:, 0:n], in0=tb[:, 0:n], in1=ts_,
                                                                                 op=ALU.mult),
                        reads=[kb, tsk], writes=[kd2])
                    P.dve(lambda e, gr=gr, psr=psr, tc_=tc_, n=n: e.tensor_tensor(out=gr, in0=psr[:, 0:n], in1=tc_,
                                                                                   op=ALU.mult),
                          reads=[pkr, tck], writes=["GR"])
                    P.dve(lambda e, gi=gi, psi=psi, tc_=tc_, n=n: e.tensor_tensor(out=gi, in0=psi[:, 0:n], in1=tc_,
                                                                                   op=ALU.mult),
                          reads=[pki, tck], writes=["GI"])
                    P.dve(lambda e, gr=gr, td2=td2, n=n: e.tensor_tensor(out=gr, in0=gr, in1=td2[:, 0:n], op=ALU.add),
                          reads=[kd2, "GR"], writes=["GR"])
                    P.dve(lambda e, gi=gi, tc2=tc2, n=n: e.tensor_tensor(out=gi, in0=gi, in1=tc2[:, 0:n], op=ALU.subtract),
                          reads=[kc2, "GI"], writes=["GI"])
                magb = V[:, MAG, col:col + 1].to_broadcast([128, T])
                for (buf, nm) in ((GR, "GR"), (GI, "GI")):
                    bv = buf[:, :] if d == 0 else buf[:, ::-1]
                    P.dve(lambda e, bv=bv, magb=magb: e.tensor_tensor_scan(out=bv, data0=magb, data1=bv, initial=0.0,
                                                                           op0=ALU.mult, op1=ALU.add),
                          reads=[nm], writes=[nm])
                if S5OPT["prefetch"] and it + 1 < len(iters):
                    prep(it + 1)
                for bi in range(4):
                    a = bi * 512
                    b0 = a + CTX if d == 0 else a
                    n = 512
                    tc_, ts_ = tsl(TC, b0, n), tsl(TS, b0, n)
                    gr, gi = GR[:, b0:b0 + n], GI[:, b0:b0 + n]
                    tk = [("tmp", i) for i in range(4)]
                    hb = hb_i % 2
                    hb_i += 1
                    P.dve(lambda e, gr=gr, tc_=tc_: e.tensor_tensor(out=tmp[0][:], in0=gr, in1=tc_, op=ALU.mult),
                          reads=["GR", tck], writes=[tk[0]])
                    P.pool(lambda e, gi=gi, ts_=ts_: e.tensor_tensor(out=tmp[1][:], in0=gi, in1=ts_, op=ALU.mult),
                           reads=["GI", tsk], writes=[tk[1]])
                    P.pool(lambda e, gr=gr, ts_=ts_: e.tensor_tensor(out=tmp[2][:], in0=gr, in1=ts_, op=ALU.mult),
                           reads=["GR", tsk], writes=[tk[2]])
                    P.dve(lambda e, gi=gi, tc_=tc_: e.tensor_tensor(out=tmp[3][:], in0=gi, in1=tc_, op=ALU.mult),
                          reads=["GI", tck], writes=[tk[3]])
                    P.dve(lambda e, hb=hb: e.tensor_tensor(out=HRb[hb][:], in0=tmp[0][:], in1=tmp[1][:],
                                                           op=ALU.subtract), reads=[tk[0], tk[1]], writes=[("HR", hb)])
                    P.dve(lambda e, hb=hb: e.tensor_tensor(out=HIb[hb][:], in0=tmp[2][:], in1=tmp[3][:], op=ALU.add),
                          reads=[tk[2], tk[3]], writes=[("HI", hb)])
                    pk, ps = nps()
                    P.mm(ps[:, :], [(LC[s][:, 0, :], HRb[hb][:]), (LC[s][:, 1, :], HIb[hb][:])],
                         reads=[lck, ("HR", hb), ("HI", hb)], writes=[pk])
                    P.dve(lambda e, a=a, ps=ps: e.tensor_tensor(out=yacc[:, a:a + 512], in0=yacc[:, a:a + 512],
                                                                in1=ps[:, :], op=ALU.add),
                          reads=[pk, "yacc"], writes=["yacc"])
                if pic == 3 and d == 1:
                    P.act(lambda e, ch=ch: e.activation(out=gT[:, ch, :], in_=yacc[:, :], func=AF.Gelu_apprx_tanh),
                          reads=["yacc"], writes=[("gT", ch)])
            P.barrier()
            P.flush()
        with ExitStack() as pb:
            B = pb.enter_context
            wgl = B(_sb(nc, "wglu", [128, 4, 512], BF16))
            sig = [B(_sb(nc, "s5sig%d" % s, [128, 512], F32)) for s in range(2)]
            sst = [B(_sb(nc, "s5st%d" % s, [128, 512], BF16)) for s in range(2)]
            nps = _rot([B(_ps(nc, "psg%d" % s, [128, 512], F32)) for s in range(4)], "psg")
            P.dma("pool", wgl[:], rows_view(G.s5_w_glu[0]), writes=["wglu"], semkey="wglu")
            gi_ = 0
            for m in range(4):
                for bi in range(4):
                    a = bi * 512
                    pk, ps = nps()
                    P.mm(ps[:, :], [(wgl[:, k, m * 128:(m + 1) * 128], gT[:, k, a:a + 512]) for k in range(4)],
                         reads=["wglu"], writes=[pk])
                    s = gi_ % 2
                    gi_ += 1
                    P.act(lambda e, s=s, ps=ps: e.activation(out=sig[s][:], in_=ps[:, :], func=AF.Sigmoid),
                          reads=[pk], writes=[("sig", s)])
                    P.dve(lambda e, s=s, m=m, a=a: e.tensor_tensor(out=sst[s][:], in0=sig[s][:], in1=gT[:, m, a:a + 512],
                                                                   op=ALU.mult),
                          reads=[("sig", s)], writes=[("sst", s)])
                    P.dma("sp", G.mixs[m, :, a:a + 512], sst[s][:], reads=[("sst", s)], writes=[("mixs", m)],
                          semkey="sst%d" % s)
            P.barrier()
            P.flush()


def _cd_mla(P, G, nc, mst):
    AX = mybir.AxisListType.X
    QSCALE = 96.0 ** -0.5
    with ExitStack() as c1:
        C = c1.enter_context
        wq = C(_sb(nc, "wq", [128, 6, 768], BF16))
        wk = C(_sb(nc, "wk", [128, 2, 512], BF16))
        wv = C(_sb(nc, "wv", [128, 2, 512], BF16))
        wst = [C(_sb(nc, "wst%d" % s, [128, 768], F32)) for s in range(2)]
        nT = C(_sb(nc, "nT", [128, 8], F32))
        grow = C(_sb(nc, "grow", [1, 192], F32))
        GQ = C(_sb(nc, "GQ", [128, 96], F32))
        GK = C(_sb(nc, "GK", [128, 96], F32))
        ropC = C(_sb(nc, "ropC", [128, NT_L, 32], F32))
        ropS = C(_sb(nc, "ropS", [128, NT_L, 32], F32))
        identb = C(_sb(nc, "identb", [128, 128], BF16))
        cqt = [C(_sb(nc, "cqt%d" % s, [128, 6, 128], BF16)) for s in range(2)]
        ckt = [C(_sb(nc, "ckt%d" % s, [128, 2, 128], BF16)) for s in range(2)]
        qf = [C(_sb(nc, "qf%d" % s, [128, 8, 96], F32)) for s in range(2)]
        kf = [C(_sb(nc, "kf%d" % s, [128, 8, 96], F32)) for s in range(2)]
        sq = [C(_sb(nc, "sqh%d" % s, [128, 8, 96], F32)) for s in range(4)]
        hs = [C(_sb(nc, "hs%d" % s, [128, 4, 8], F32)) for s in range(4)]
        R1 = [C(_sb(nc, "R1_%d" % s, [128, 8, 32], F32)) for s in range(4)]
        R2 = [C(_sb(nc, "R2_%d" % s, [128, 8, 32], F32)) for s in range(4)]
        xb = [C(_sb(nc, "xb%d" % s, [128, 8, 96], BF16)) for s in range(4)]
        xst = [C(_sb(nc, "xst%d" % s, [128, 8, 128], BF16)) for s in range(4)]
        vst = [C(_sb(nc, "vst%d" % s, [128, 8, 65], BF16)) for s in range(2)]
        psf = [C(_ps(nc, "psm%d" % s, [128, 512], F32)) for s in range(5)]
        ptb = [C(_ps(nc, "pst%d" % s, [128, 1024], BF16)) for s in range(2)]
        nps = _rot([psf[4]], "psm4")
        P.dma("sp", nT[:, 0:6], G.mla_q_norm[0].rearrange("(k p) -> p k", p=128), writes=["nT"], semkey="nT",
              allow_slow_non_contiguous=True)
        P.dma("sp", nT[:, 6:8], G.mla_kv_norm[0].rearrange("(k p) -> p k", p=128), writes=["nT"], semkey="nT",
              allow_slow_non_contiguous=True)
        wi = 0
        jobs = [(G.mla_w_uq[0, k * 128:(k + 1) * 128, :], wq[:, k, :], 768, k) for k in range(6)]
        jobs += [(G.mla_w_uk[0, k * 128:(k + 1) * 128, :], wk[:, k, :], 512, 6 + k) for k in range(2)]
        jobs += [(G.mla_w_uv[0, k * 128:(k + 1) * 128, :], wv[:, k, :], 512, 6 + k) for k in range(2)]
        for (src, dst, n, nc_) in jobs:
            s = wi % 2
            wi += 1
            P.dma("sp", wst[s][:, 0:n], src, writes=[("wst", s)], semkey="wst%d" % s)
            P.dve(lambda e, s=s, dst=dst, n=n, nc_=nc_: e.tensor_scalar(out=dst, in0=wst[s][:, 0:n],
                                                                         scalar1=nT[:, nc_:nc_ + 1], scalar2=None,
                                                                         op0=ALU.mult),
                  reads=[("wst", s), "nT"], writes=["wqkv"])
        P.dma("sp", grow[0:1, 0:96], G.mla_qn_gain[0:1, :], writes=["grow"], semkey="grow")
        P.dma("sp", grow[0:1, 96:192], G.mla_kn_gain[0:1, :], writes=["grow"], semkey="grow")
        pk, ps = nps()
        P.mm(ps[:, 0:192], [(G.ones[0:1, 0:128], grow[0:1, :])], reads=["grow"], writes=[pk])
        P.act(lambda e, ps=ps: e.activation(out=GQ[:, :], in_=ps[:, 0:96], func=AF.Identity, scale=QSCALE),
              reads=[pk], writes=["GQ"])
        P.act(lambda e, ps=ps: e.activation(out=GK[:, :], in_=ps[:, 96:192], func=AF.Identity),
              reads=[pk], writes=["GK"])
        P.dma("sp", ropC[:], G.ropC.rearrange("(t p) f -> p t f", p=128), writes=["rop"], semkey="ropC")
        P.dma("sp", ropS[:], G.ropS.rearrange("(t p) f -> p t f", p=128), writes=["rop"], semkey="ropS")
        P.dve(lambda e: e.tensor_copy(out=identb[:], in_=G.ident[:]), writes=["identb"])
        for s in range(2):
            P.pool(lambda e, s=s: e.memset(vst[s][:, :, 64:65], 1.0), writes=[("vst", s)])

        def norm_rope(src, sk, gain, gk, dst, dk, t, rope, w):
            sq_, hs_, R1_, R2_ = sq[w], hs[w], R1[w], R2[w]
            P.pool(lambda e: e.tensor_tensor(out=sq_[:], in0=src[:], in1=src[:], op=ALU.mult), reads=[sk],
                   writes=[("sq", w)])
            yield
            P.dve(lambda e: e.tensor_reduce(out=hs_[:, 0, :], in_=sq_[:], axis=AX, op=ALU.add), reads=[("sq", w)],
                  writes=[("hs", w)])
            yield
            P.dve(lambda e: e.tensor_scalar(out=hs_[:, 1, :], in0=hs_[:, 0, :], scalar1=1.0 / 96, scalar2=EPS,
                                            op0=ALU.mult, op1=ALU.add), reads=[("hs", w)], writes=[("hs", w)])
            yield
            P.act(lambda e: e.activation(out=hs_[:, 2, :], in_=hs_[:, 1, :], func=AF.Sqrt), reads=[("hs", w)],
                  writes=[("hs", w)])
            yield
            P.dve(lambda e: e.reciprocal(out=hs_[:, 3, :], in_=hs_[:, 2, :]), reads=[("hs", w)], writes=[("hs", w)])
            yield
            P.dve(lambda e: e.tensor_tensor(out=src[:], in0=src[:],
                                            in1=hs_[:, 3, :].unsqueeze(2).to_broadcast([128, 8, 96]), op=ALU.mult),
                  reads=[sk, ("hs", w)], writes=[sk])
            yield
            P.dve(lambda e: e.tensor_tensor(out=src[:], in0=src[:],
                                            in1=gain[:, :].unsqueeze(1).to_broadcast([128, 8, 96]), op=ALU.mult),
                  reads=[sk, gk], writes=[sk])
            yield
            if rope:
                P.dve(lambda e: e.tensor_tensor(out=R1_[:], in0=src[:, :, 64:96],
                                                in1=ropC[:, t, :].unsqueeze(1).to_broadcast([128, 8, 32]), op=ALU.mult),
                      reads=[sk, "rop"], writes=[("R1", w)])
                yield
                for (o0, i0) in ((0, 72), (8, 64), (16, 88), (24, 80)):
                    P.dve(lambda e, o0=o0, i0=i0: e.tensor_tensor(
                        out=R2_[:, :, o0:o0 + 8], in0=src[:, :, i0:i0 + 8],
                        in1=ropS[:, t, o0:o0 + 8].unsqueeze(1).to_broadcast([128, 8, 8]), op=ALU.mult),
                        reads=[sk, "rop"], writes=[("R2", w)])
                yield
                P.dve(lambda e: e.tensor_tensor(out=dst[:, :, 64:96], in0=R1_[:], in1=R2_[:], op=ALU.add),
                      reads=[("R1", w), ("R2", w)], writes=[dk])
                P.act(lambda e: e.activation(out=dst[:, :, 0:64], in_=src[:, :, 0:64], func=AF.Identity),
                      reads=[sk], writes=[dk])
                yield
            else:
                P.act(lambda e: e.activation(out=dst[:], in_=src[:], func=AF.Identity), reads=[sk], writes=[dk])
                yield

        def transpose_store(srcb, sbk, stt, stk, dram_view, semk, w):
            pk, pt = ("pst", w), ptb[w]
            for h in range(8):
                P.tr(pt[0:96, h * 128:(h + 1) * 128], srcb[:, h, :], identb[:], reads=[sbk, "identb"], writes=[pk])
            yield
            P.dve(lambda e: e.tensor_copy(out=stt[0:96, :, :], in_=pt[0:96, :].rearrange("p (h n) -> p h n", h=8)),
                  reads=[pk], writes=[stk])
            yield
            P.dma("sp", dram_view, stt[0:96, :, :], reads=[stk], writes=["dram_qk"], semkey=semk)
            yield

        def k_chain(t):
            s = t % 2
            w = 0
            rkv = mst[:, t, 4:5]
            pkk, psk = ("psm", 0), psf[0]
            P.mm(psk[:, :], [(ckt[s][:, k, :], wk[:, k, :]) for k in range(2)], reads=[("ckt", s), "wqkv"], writes=[pkk])
            pkv, psv = ("psm", 1), psf[1]
            P.mm(psv[:, :], [(ckt[s][:, k, :], wv[:, k, :]) for k in range(2)], reads=[("ckt", s), "wqkv"], writes=[pkv])
            yield
            P.act(lambda e: e.activation(out=vst[s][:, :, 0:64], in_=psv[:, :].rearrange("p (h e) -> p h e", h=8),
                                         func=AF.Identity, scale=rkv), reads=[pkv], writes=[("vst", s)])
            P.dma("sp", G.vs[t], vst[s][:], reads=[("vst", s)], writes=["dram_v"], semkey="vst%d" % s)
            yield
            P.act(lambda e: e.activation(out=kf[s][:, :, 0:64], in_=psk[:, :].rearrange("p (h e) -> p h e", h=8),
                                         func=AF.Identity, scale=rkv), reads=[pkk], writes=[("kf", s)])
            P.dve(lambda e: e.tensor_copy(out=kf[s][:, :, 64:96],
                                          in_=mst[:, t, 8:40].unsqueeze(1).to_broadcast([128, 8, 32])),
                  writes=[("kf", s)])
            yield
            yield from norm_rope(kf[s], ("kf", s), GK, "GK", xb[2 * w + s], ("xb", 2 * w + s), t, t < NT_L, 2 * w + s)
            yield from transpose_store(xb[2 * w + s], ("xb", 2 * w + s), xst[2 * w + s], ("xst", 2 * w + s),
                                       G.ks[:, :, t * 128:(t + 1) * 128].rearrange("h d n -> d h n"),
                                       "xst%d" % (2 * w + s), w)

        def q_chain(t):
            s = t % 2
            w = 1
            rq = mst[:, t, 3:4]
            qfl = qf[s][:].rearrange("p h e -> p (h e)")
            pka, psa = ("psm", 2), psf[2]
            P.mm(psa[:, :], [(cqt[s][:, k, :], wq[:, k, 0:512]) for k in range(6)], reads=[("cqt", s), "wqkv"],
                 writes=[pka])
            pkb, psb = ("psm", 3), psf[3]
            P.mm(psb[:, 0:256], [(cqt[s][:, k, :], wq[:, k, 512:768]) for k in range(6)],
                 reads=[("cqt", s), "wqkv"], writes=[pkb])
            yield
            P.act(lambda e: e.activation(out=qfl[:, 0:512], in_=psa[:, :], func=AF.Identity, scale=rq),
                  reads=[pka], writes=[("qf", s)])
            P.act(lambda e: e.activation(out=qfl[:, 512:768], in_=psb[:, 0:256], func=AF.Identity, scale=rq),
                  reads=[pkb], writes=[("qf", s)])
            yield
            yield from norm_rope(qf[s], ("qf", s), GQ, "GQ", xb[2 * w + s], ("xb", 2 * w + s), t, True, 2 * w + s)
            yield from transpose_store(xb[2 * w + s], ("xb", 2 * w + s), xst[2 * w + s], ("xst", 2 * w + s),
                                       G.qs[:, :, t * 128:(t + 1) * 128].rearrange("h d n -> d h n"),
                                       "xst%d" % (2 * w + s), w)

        def c1_loads(t):
            s = t % 2
            P.dma("pool", ckt[s][:], G.zs[10:12, :, t * 128:(t + 1) * 128].rearrange("j p n -> p j n"),
                  writes=[("ckt", s)], semkey="ckt%d" % s)
            if t < NT_L:
                P.dma("pool", cqt[s][:], G.zs[4:10, :, t * 128:(t + 1) * 128].rearrange("j p n -> p j n"),
                      writes=[("cqt", s)], semkey="cqt%d" % s)

        c1_loads(0)
        for t in range(NT):
            if t + 1 < NT:
                c1_loads(t + 1)
            run_zip([k_chain(t)] + ([q_chain(t)] if t < NT_L else []), 2)
        P.barrier()
        P.flush()
    with ExitStack() as c2:
        C = c2.enter_context
        qh = [C(_sb(nc, "qh%d" % s, [128, SEQ], BF16)) for s in range(2)]
        kh = [C(_sb(nc, "kh%d" % s, [128, T_ALL], BF16)) for s in range(2)]
        vh = [C(_sb(nc, "vh%d" % s, [128, NT, 65], BF16)) for s in range(2)]
        pT = [C(_sb(nc, "pT%d" % s, [128, 512], BF16)) for s in range(3)]
        otok = C(_sb(nc, "otok", [128, NT_L, 512], BF16))
        rec = C(_sb(nc, "rec", [128, 8], F32))
        identb = C(_sb(nc, "identb2", [128, 128], BF16))
        ost = [C(_sb(nc, "ost%d" % s, [128, 4, 128], BF16)) for s in range(2)]
        psS = [C(_ps(nc, "psS%d" % s, [128, 512], F32)) for s in range(3)]
        psO = [C(_ps(nc, "psO%d" % s, [128, 512], F32)) for s in range(4)]
        npt = _rot([C(_ps(nc, "pst2_%d" % s, [128, 1024], BF16)) for s in range(1)], "pst2")
        P.dve(lambda e: e.tensor_copy(out=identb[:], in_=G.ident[:]), writes=["identb"])
        steps = [(h, qb_, kc) for h in range(8) for qb_ in range(4) for kc in range(NT)]
        ri = [0]

        def head_loads(h):
            s = h % 2
            P.dma("sp", qh[s][0:96, :], G.qs[h], writes=[("qh", s)], semkey="qh%d" % s)
            P.dma("sp", kh[s][0:96, :], G.ks[h], writes=[("kh", s)], semkey="kh%d" % s)
            P.dma("sp", vh[s][:], G.vs[:, :, h, :].rearrange("t p e -> p t e"), writes=[("vh", s)],
                  semkey="vh%d" % s)

        def emit_S(i):
            h, qb_, kc = steps[i]
            s = h % 2
            if h == 0 and qb_ == 0 and kc == 0:
                head_loads(0)
            if qb_ == 0 and kc == 6 and h + 1 < 8:
                head_loads(h + 1)
            ss = i % 3
            P.mm(psS[ss][:, :], [(kh[s][0:96, kc * 128:(kc + 1) * 128], qh[s][0:96, qb_ * 512:(qb_ + 1) * 512])],
                 reads=[("qh", s), ("kh", s)], writes=[("psS", ss)])

        def emit_rest(i):
            h, qb_, kc = steps[i]
            s = h % 2
            ss = i % 3
            ps_ = i % 3
            P.act(lambda e: e.activation(out=pT[ps_][:], in_=psS[ss][:, :], func=AF.Exp),
                  reads=[("psS", ss)], writes=[("pT", ps_)])
            for j in range(4):
                wr = [("psO", j)] if kc in (0, NT - 1) else []
                Prog_mm1(P, psO[j][:, 0:65], pT[ps_][:, j * 128:(j + 1) * 128], vh[s][:, kc, :],
                         start=(kc == 0), stop=(kc == NT - 1), reads=[("pT", ps_), ("vh", s)], writes=wr)
            if kc == NT - 1:
                for j in range(4):
                    r_ = ri[0] % 8
                    ri[0] += 1
                    t = qb_ * 4 + j
                    P.dve(lambda e, j=j, r_=r_: e.reciprocal(out=rec[:, r_:r_ + 1], in_=psO[j][:, 64:65]),
                          reads=[("psO", j)], writes=[("rec", r_)])
                    P.act(lambda e, j=j, r_=r_, t=t, h=h: e.activation(out=otok[:, t, h * 64:(h + 1) * 64],
                                                                      in_=psO[j][:, 0:64], func=AF.Identity,
                                                                      scale=rec[:, r_:r_ + 1]),
                          reads=[("psO", j), ("rec", r_)], writes=["otok"])

        emit_S(0)
        emit_S(1)
        for i in range(len(steps)):
            if i + 2 < len(steps):
                emit_S(i + 2)
            emit_rest(i)
        for t in range(NT_L):
            s = t % 2
            pk, pt = npt()
            for c in range(4):
                P.tr(pt[:, c * 128:(c + 1) * 128], otok[:, t, c * 128:(c + 1) * 128], identb[:], reads=["otok", "identb"],
                     writes=[pk])
            P.dve(lambda e, s=s, pt=pt: e.tensor_copy(out=ost[s][:], in_=pt[:, 0:512].rearrange("p (c n) -> p c n", c=4)),
                  reads=[pk], writes=[("ost", s)])
            P.dma("sp", G.mixs[4:8, :, t * 128:(t + 1) * 128].rearrange("j p n -> p j n"), ost[s][:],
                  reads=[("ost", s)], writes=["mixs_o"], semkey="ost%d" % s)
        P.barrier()
        P.flush()
```

```python
import math
from contextlib import ExitStack
from types import SimpleNamespace

import numpy as np
import concourse.bass as bass
import concourse.mybir as mybir
from concourse.bass_utils import run_bass_kernel_spmd

F32 = mybir.dt.float32
BF16 = mybir.dt.bfloat16
I32 = mybir.dt.int32
AF = mybir.ActivationFunctionType
ALU = mybir.AluOpType

D = 1024
SEQ = 2048
CTX = 256
NT_L = 16
NT = 18
T_ALL = NT * 128
EPS = 1e-6
NEXP = 32
DEXP = 512
BIG = 1.0e30


_UID = [0]


def _sb(nc, name, shape, dt):
    _UID[0] += 1
    return nc.sbuf_tensor("%s_u%d" % (name, _UID[0]), shape, dt)


def _ps(nc, name, shape, dt):
    _UID[0] += 1
    return nc.psum_tensor("%s_u%d" % (name, _UID[0]), shape, dt)


class _Eng:
    def __init__(self, name, sem):
        self.name = name
        self.sem = sem
        self.tick = 0
        self.ins = []
        self.sim = []
        self.waited = {}


class Prog:
    def __init__(self, nc, enter):
        self.nc = nc
        self.enter = enter
        self.eng = {n: _Eng(n, enter(nc.semaphore("s_" + n))) for n in ("pe", "act", "dve", "pool", "sp")}
        self.res = {}
        self.dsem = {}
        self.ps_i = 0

    def _wait(self, X, ev):
        if ev is None:
            return
        kind, k, v = ev
        key = (kind, k)
        if X.waited.get(key, 0) >= v:
            return
        X.waited[key] = v
        sem = self.eng[k].sem if kind == "e" else self.dsem[k][0]
        X.ins.append(lambda e, sem=sem, v=v: e.wait_ge(sem, v))
        X.sim.append(("w", key, v))

    def _deps(self, X, reads, writes):
        for r in reads:
            st = self.res.get(r)
            if st:
                self._wait(X, st[0])
        for w in writes:
            st = self.res.get(w)
            if st:
                self._wait(X, st[0])
                for ev in st[1].values():
                    self._wait(X, ev)

    def _commit(self, ev, reads, writes, rkey):
        for r in reads:
            st = self.res.setdefault(r, [None, {}])
            st[1][rkey] = ev
        for w in writes:
            self.res[w] = [ev, {}]

    def op(self, en, fn, reads=(), writes=()):
        X = self.eng[en]
        self._deps(X, reads, writes)
        X.tick += 1
        sem = X.sem
        X.ins.append(lambda e, fn=fn, sem=sem: fn(e).then_inc(sem, 1))
        X.sim.append(("i", ("e", en), 1))
        self._commit(("e", en, X.tick), reads, writes, en)

    def act(self, fn, reads=(), writes=()):
        self.op("act", fn, reads, writes)

    def dve(self, fn, reads=(), writes=()):
        self.op("dve", fn, reads, writes)

    def pool(self, fn, reads=(), writes=()):
        self.op("pool", fn, reads, writes)

    def mm(self, out, pairs, reads=(), writes=()):
        X = self.eng["pe"]
        self._deps(X, reads, writes)
        n = len(pairs)
        sem = X.sem
        for i, (l, r) in enumerate(pairs):
            if i == n - 1:
                X.ins.append(lambda e, l=l, r=r, i=i: e.matmul(out, l, r, start=(i == 0), stop=True).then_inc(sem, 1))
            else:
                X.ins.append(lambda e, l=l, r=r, i=i: e.matmul(out, l, r, start=(i == 0), stop=False))
        X.tick += 1
        X.sim.append(("i", ("e", "pe"), 1))
        self._commit(("e", "pe", X.tick), reads, writes, "pe")

    def tr(self, out, in_, ident, reads=(), writes=()):
        self.op("pe", lambda e: e.transpose(out, in_, ident), reads, writes)

    def dma(self, q, out, in_, reads=(), writes=(), semkey=None, **kw):
        X = self.eng[q]
        self._deps(X, reads, writes)
        if semkey not in self.dsem:
            self.dsem[semkey] = [self.enter(self.nc.semaphore("d%d" % len(self.dsem))), 0]
        d = self.dsem[semkey]
        d[1] += 16
        sem = d[0]
        X.ins.append(lambda e: e.dma_start(out=out, in_=in_, **kw).then_inc(sem, 16))
        X.sim.append(("i", ("d", semkey), 16))
        self._commit(("d", semkey, d[1]), reads, writes, ("d", semkey))

    def barrier(self):
        evs = [("e", n, E.tick) for n, E in self.eng.items() if E.tick > 0]
        evs += [("d", k, d[1]) for k, d in self.dsem.items()]
        for X in self.eng.values():
            for ev in evs:
                self._wait(X, ev)
        self.res = {}

    def simulate(self):
        if not hasattr(self, "semval"):
            self.semval = {}
        pos = {n: 0 for n in self.eng}
        while True:
            prog = False
            for n, E in self.eng.items():
                while pos[n] < len(E.sim):
                    kind, key, v = E.sim[pos[n]]
                    if kind == "w":
                        if self.semval.get(key, 0) < v:
                            break
                    else:
                        self.semval[key] = self.semval.get(key, 0) + v
                    pos[n] += 1
                    prog = True
            if not prog:
                break
        stuck = {n: (pos[n], len(E.sim), E.sim[pos[n]], self.semval.get(E.sim[pos[n]][1], 0))
                 for n, E in self.eng.items() if pos[n] < len(E.sim)}
        if stuck:
            raise RuntimeError("DEADLOCK in recorded program: %r" % (stuck,))
        for E in self.eng.values():
            E.sim = []

    def flush(self):
        self.simulate()
        with self.nc.Block() as blk:
            def mk(name):
                lst = self.eng[name].ins

                def run(e):
                    for f in lst:
                        f(e)
                return run
            blk.tensor(mk("pe"))
            blk.scalar(mk("act"))
            blk.vector(mk("dve"))
            blk.gpsimd(mk("pool"))
            blk.sync(mk("sp"))
        for E in self.eng.values():
            E.ins = []


def run_zip(gens, width):
    pend = list(gens)
    act = []
    while pend or act:
        while pend and len(act) < width:
            act.append(pend.pop(0))
        for g in list(act):
            try:
                next(g)
            except StopIteration:
                act.remove(g)


def rows_view(ap2d):
    return ap2d.rearrange("(k p) n -> p k n", p=128)


def phase_init(P, G):
    nc = P.nc
    P.dma("sp", G.xl[:, 0:NT_L, :], G.x.rearrange("(t p) d -> p t d", p=128),
          writes=[("xl", t) for t in range(NT_L)], semkey="ldx")
    P.dma("sp", G.xl[:, NT_L:NT, :], G.ctx.rearrange("(t p) d -> p t d", p=128),
          writes=[("xl", t) for t in range(NT_L, NT)], semkey="ldc")
    P.pool(lambda e: e.iota(G.iot[:, 0:128], [[1, 128]], base=0, channel_multiplier=-1), writes=["iot"])
    P.pool(lambda e: e.tensor_single_scalar(out=G.ident[:], in_=G.iot[:, 0:128], scalar=0, op=ALU.is_equal),
           reads=["iot"], writes=["ident"])
    P.pool(lambda e: e.memset(G.ones[:], 1.0), writes=["ones"])
    P.pool(lambda e: e.memset(G.onesb[:], 1.0), writes=["onesb"])


def phase_ada(P, G, i, blocks):
    nc = P.nc
    with ExitStack() as ph:
        A = ph.enter_context
        wblk = [A(_sb(nc, "adaw%d" % s, [128, 8, 512], F32)) for s in range(2)]
        brow = [A(_sb(nc, "adab%d" % s, [1, 512], F32)) for s in range(2)]
        crow = A(_sb(nc, "crow", [2, 1024], F32))
        scT = A(_sb(nc, "scT", [128, 8, 2], F32))
        rowb = A(_sb(nc, "rowb", [1, 2, 512], F32))
        pst = [A(_ps(nc, "psa%d" % s, [128, 512], F32)) for s in range(4)]
        P.dma("sp", crow[0:1, :], G.c[0:1, :], writes=["crow"], semkey="crow")
        P.dma("sp", crow[1:2, :], G.c_ctx[0:1, :], writes=["crow"], semkey="crow")
        P.act(lambda e: e.activation(out=crow[:], in_=crow[:], func=AF.Silu), reads=["crow"], writes=["crow"])
        for k in range(8):
            P.mm(pst[0][:, 2 * k:2 * k + 2], [(crow[0:2, k * 128:(k + 1) * 128], G.ident[0:2, 0:2])],
                 reads=["crow", "ident"], writes=[("psa", 0)])
        P.dve(lambda e: e.tensor_copy(out=scT[:].rearrange("p k r -> p (k r)"), in_=pst[0][:, 0:16]),
              reads=[("psa", 0)], writes=["scT"])
        pi = 1
        for bi, blk in enumerate(blocks):
            s = bi % 2
            which, half = blk // 2, blk % 2
            P.dma("sp", wblk[s][:], rows_view(G.ada_w[i, :, blk * 512:(blk + 1) * 512]),
                  writes=[("adaw", s)], semkey="adaw%d" % s)
            P.dma("sp", brow[s][:], G.ada_b[i:i + 1, blk * 512:(blk + 1) * 512],
                  writes=[("adab", s)], semkey="adab%d" % s)
            if which in (2, 5):
                gt = G.g1b if which == 2 else G.g2b
                for r in range(2):
                    pk = pi % 4
                    pi += 1
                    pairs = [(scT[:, k, r:r + 1], wblk[s][:, k, :]) for k in range(8)]
                    pairs.append((G.ones[0:1, 0:1], brow[s][0:1, :]))
                    P.mm(pst[pk][0:1, :], pairs, reads=["scT", ("adaw", s), ("adab", s), "ones"], writes=[("psa", pk)])
                    P.dve(lambda e, pk=pk, r=r: e.tensor_copy(out=rowb[0:1, r, :], in_=pst[pk][0:1, :]),
                          reads=[("psa", pk)], writes=[("rowb", r)])
                    pk2 = pi % 4
                    pi += 1
                    P.mm(pst[pk2][:, :], [(G.ones[0:1, 0:128], rowb[0:1, r, :])],
                         reads=[("rowb", r), "ones"], writes=[("psa", pk2)])
                    P.act(lambda e, pk2=pk2, r=r, gt=gt, half=half: e.activation(
                        out=gt[:, r, half * 512:(half + 1) * 512], in_=pst[pk2][:, :], func=AF.Identity),
                        reads=[("psa", pk2)], writes=[("gt", which, r)])
            else:
                for f in range(4):
                    pk = pi % 4
                    pi += 1
                    pairs = [(wblk[s][:, k, f * 128:(f + 1) * 128], scT[:, k, :]) for k in range(8)]
                    pairs.append((brow[s][0:1, f * 128:(f + 1) * 128], G.ones[0:1, 0:2]))
                    P.mm(pst[pk][:, 0:2], pairs, reads=["scT", ("adaw", s), ("adab", s), "ones"], writes=[("psa", pk)])
                    addc = 1.0 if which in (1, 4) else 0.0
                    P.dve(lambda e, pk=pk, f=f, which=which, half=half, addc=addc: e.tensor_scalar(
                        out=G.modT[:, which, half * 4 + f, :], in0=pst[pk][:, 0:2], scalar1=addc, scalar2=None,
                        op0=ALU.add), reads=[("psa", pk)], writes=["modT"])
        P.barrier()
        P.flush()


def modulate(P, G, hT, wsh, wsc, ntiles, scr, router=None):
    def tile_gen(t):
        r = 0 if t < NT_L else 1
        s = t % 2
        xn = scr.xn[s]
        P.act(lambda e: e.activation(out=scr.junk[s][:], in_=G.xl[:, t, :], func=AF.Square,
                                     accum_out=scr.st[:, t, 0:1]),
              reads=[("xl", t)], writes=[("junk", s), ("st", t)])
        yield
        P.dve(lambda e: e.tensor_scalar(out=scr.st[:, t, 1:2], in0=scr.st[:, t, 0:1], scalar1=1.0 / D,
                                        scalar2=EPS, op0=ALU.mult, op1=ALU.add),
              reads=[("st", t)], writes=[("st1", t)])
        yield
        P.act(lambda e: e.activation(out=scr.st[:, t, 2:3], in_=scr.st[:, t, 1:2], func=AF.Sqrt),
              reads=[("st1", t)], writes=[("st2", t)])
        yield
        P.dve(lambda e: e.reciprocal(out=scr.st[:, t, 3:4], in_=scr.st[:, t, 2:3]),
              reads=[("st2", t)], writes=[("st3", t)])
        yield
        P.dve(lambda e: e.tensor_scalar(out=xn[:], in0=G.xl[:, t, :], scalar1=scr.st[:, t, 3:4],
                                        scalar2=None, op0=ALU.mult),
              reads=[("xl", t), ("st3", t)], writes=[("xn", s)])
        yield
        for hb in range(2):
            pk, ps = scr.next_ps()
            for j in range(4):
                c = hb * 4 + j
                P.tr(ps[:, j * 128:(j + 1) * 128], xn[:, c * 128:(c + 1) * 128], G.ident[:],
                     reads=[("xn", s), "ident"], writes=[pk])
            yield
            for j in range(4):
                c = hb * 4 + j
                if router is None:
                    dst = hT[:, c, t * 128:(t + 1) * 128]
                    wr = [("hT", t, c)]
                else:
                    dst = scr.t32[s][:, c, :]
                    wr = [("t32", s, c)]
                P.act(lambda e, dst=dst, ps=ps, j=j, c=c: e.activation(
                    out=dst, in_=ps[:, j * 128:(j + 1) * 128], func=AF.Identity,
                    scale=G.modT[:, wsc, c, r:r + 1], bias=G.modT[:, wsh, c, r:r + 1]),
                    reads=[pk, "modT"], writes=wr)
            yield
        if router is not None:
            P.pool(lambda e: e.tensor_copy(out=hT[:, :, t * 128:(t + 1) * 128], in_=scr.t32[s][:]),
                   reads=[("t32", s, c) for c in range(8)], writes=[("hT", t, c) for c in range(8)])
            yield
            router(t, s)
            yield
    run_zip([tile_gen(t) for t in range(ntiles)], 2)


def make_scr(P, A, nps, with_t32=False):
    nc = P.nc
    scr = SimpleNamespace()
    scr.junk = [A(_sb(nc, "junk%d" % s, [128, 1024], BF16)) for s in range(2)]
    scr.xn = [A(_sb(nc, "xn%d" % s, [128, 1024], F32)) for s in range(2)]
    scr.st = A(_sb(nc, "mstat", [128, NT, 4], F32))
    if with_t32:
        scr.t32 = [A(_sb(nc, "t32_%d" % s, [128, 8, 128], F32)) for s in range(2)]
    scr.ps = [A(_ps(nc, "psm%d" % s, [128, 512], F32)) for s in range(nps)]
    scr.i = 0

    def next_ps():
        k = scr.i % nps
        scr.i += 1
        return ("psm", k), scr.ps[k]
    scr.next_ps = next_ps
    return scr


def resid_update(P, G, t, ps, pk, gt, c0, n, tmp, tk, gate=None):
    r = 0 if t < NT_L else 1
    if gate is None:
        P.dve(lambda e: e.tensor_tensor(out=tmp[:, 0:n], in0=ps, in1=gt[:, r, c0:c0 + n], op=ALU.mult),
              reads=[pk, ("gt", r)], writes=[tk])
    else:
        P.dve(lambda e: e.scalar_tensor_tensor(out=tmp[:, 0:n], in0=ps, scalar=gate, in1=gt[:, r, c0:c0 + n],
                                               op0=ALU.mult, op1=ALU.mult),
              reads=[pk, ("gt", r), "gates"], writes=[tk])
    P.dve(lambda e: e.tensor_tensor(out=G.xl[:, t, c0:c0 + n], in0=G.xl[:, t, c0:c0 + n], in1=tmp[:, 0:n],
                                    op=ALU.add),
          reads=[tk, ("xl", t)], writes=[("xl", t)])


TOKBLK = [(0, 512), (512, 512), (1024, 512), (1536, 512), (2048, 256)]


def phase_mixer_ab(P, G):
    nc = P.nc
    with ExitStack() as ph:
        A = ph.enter_context
        hT = A(_sb(nc, "hT", [128, 8, T_ALL], BF16))
        ntb = 5
        with ExitStack() as p1:
            scr = make_scr(P, p1.enter_context, 4)
            modulate(P, G, hT, 0, 1, NT, scr)
            P.barrier()
            P.flush()
        with ExitStack() as p2:
            B = p2.enter_context
            ybT = B(_sb(nc, "ybT", [128, 8, T_ALL], BF16))
            wB = [B(_sb(nc, "wB%d" % s, [128, 8, 3, 128], BF16)) for s in range(2)]
            prod = B(_sb(nc, "prod", [128, 2308], F32))
            tmpc = [B(_sb(nc, "tmpc%d" % s, [128, 512], F32)) for s in range(2)]
            cv = [B(_sb(nc, "cv%d" % s, [128, 512], F32)) for s in range(2)]
            cw3 = prod[0:3, 0:1024]
            cwT = B(_sb(nc, "cwT", [128, 8, 3], F32))
            woB = B(_sb(nc, "woB", [128, 8, 1024], BF16))
            tmpr = [B(_sb(nc, "tmpr%d" % s, [128, 512], F32)) for s in range(2)]
            pss = [B(_ps(nc, "psb%d" % s, [128, 512], F32)) for s in range(8)]
            psi = [0]

            def nps():
                k = psi[0] % 8
                psi[0] += 1
                return ("psb", k), pss[k]
            P.dma("sp", cw3, G.conv_w[0, :, :], writes=["prod"], semkey="cw3")
            pk, ps = nps()
            for c in range(8):
                P.tr(ps[:, c * 3:c * 3 + 3], prod[0:3, c * 128:(c + 1) * 128], G.ident[0:3, 0:3],
                     reads=["prod", "ident"], writes=[pk])
            P.dve(lambda e: e.tensor_copy(out=cwT[:].rearrange("p c k -> p (c k)"), in_=ps[:, 0:24]),
                  reads=[pk], writes=["cwT"])
            P.dma("pool", woB[:], rows_view(G.ab_w_out[0, 1024:2048, :]), writes=["woB"], semkey="woB")
            P.dve(lambda e: e.memset(prod[:], 0.0), reads=["cwT"], writes=["prod"])

            def poff(a):
                return 1 + a if a < SEQ else 2051 + (a - SEQ)
            for c in range(8):
                s = c % 2
                for j in range(3):
                    col = 2048 + j * 1024 + c * 128
                    P.dma("pool", wB[s][:, :, j, :], rows_view(G.ab_w_in[0, :, col:col + 128]),
                          writes=[("wB", s, j)], semkey="wB%d_%d" % (s, j))
                for bi, (a, n) in enumerate(TOKBLK):
                    pkc, psc = nps()
                    P.mm(psc[:, 0:n], [(wB[s][:, k, 1, :], hT[:, k, a:a + n]) for k in range(8)],
                         reads=[("wB", s, 1), "hTall"], writes=[pkc])
                    pkx, psx = nps()
                    P.mm(psx[:, 0:n], [(wB[s][:, k, 2, :], hT[:, k, a:a + n]) for k in range(8)],
                         reads=[("wB", s, 2), "hTall"], writes=[pkx])
                    ts = bi % 2
                    P.act(lambda e, ts=ts, psc=psc, n=n: e.activation(out=tmpc[ts][:, 0:n], in_=psc[:, 0:n],
                                                                      func=AF.Identity),
                          reads=[pkc], writes=[("tmpc", ts)])
                    o = poff(a)
                    P.dve(lambda e, ts=ts, psx=psx, n=n, o=o: e.tensor_tensor(
                        out=prod[:, o:o + n], in0=tmpc[ts][:, 0:n], in1=psx[:, 0:n], op=ALU.mult),
                        reads=[("tmpc", ts), pkx], writes=["prod"])
                for bi, (a, n) in enumerate(TOKBLK):
                    pkb, psb = nps()
                    P.mm(psb[:, 0:n], [(wB[s][:, k, 0, :], hT[:, k, a:a + n]) for k in range(8)],
                         reads=[("wB", s, 0), "hTall"], writes=[pkb])
                    ts = bi % 2
                    o = poff(a)
                    P.act(lambda e, ts=ts, n=n, o=o, c=c: e.activation(
                        out=cv[ts][:, 0:n], in_=prod[:, o - 1:o - 1 + n], func=AF.Identity,
                        scale=cwT[:, c, 0:1]), reads=["prod", "cwT"], writes=[("cv", ts)])
                    for kk in (1, 2):
                        P.dve(lambda e, ts=ts, n=n, o=o, c=c, kk=kk: e.scalar_tensor_tensor(
                            out=cv[ts][:, 0:n], in0=prod[:, o - 1 + kk:o - 1 + kk + n], scalar=cwT[:, c, kk:kk + 1],
                            in1=cv[ts][:, 0:n], op0=ALU.mult, op1=ALU.add),
                            reads=["prod", "cwT", ("cv", ts)], writes=[("cv", ts)])
                    P.dve(lambda e, ts=ts, n=n, a=a, c=c, psb=psb: e.tensor_tensor(
                        out=ybT[:, c, a:a + n], in0=cv[ts][:, 0:n], in1=psb[:, 0:n], op=ALU.mult),
                        reads=[("cv", ts), pkb], writes=["ybT"])
            ui = 0
            for t in range(NT):
                for db in range(2):
                    pky, psy = nps()
                    P.mm(psy[:, :], [(ybT[:, k, t * 128:(t + 1) * 128], woB[:, k, db * 512:(db + 1) * 512])
                                     for k in range(8)], reads=["ybT", "woB"], writes=[pky])
                    resid_update(P, G, t, psy[:, :], pky, G.g1b, db * 512, 512, tmpr[ui % 2], ("tmpr", ui % 2))
                    ui += 1
            P.barrier()
            P.flush()
        with ExitStack() as p3:
            B = p3.enter_context
            wU = B(_sb(nc, "wU", [128, 8, 1024], BF16))
            wV = B(_sb(nc, "wV", [128, 8, 1024], BF16))
            woA = B(_sb(nc, "woA", [128, 8, 1024], BF16))
            guT = B(_sb(nc, "guT", [128, 8, 512], BF16))
            gv = B(_sb(nc, "gv", [128, 1024], F32))
            vc = [B(_sb(nc, "vc%d" % s, [128, 1024], BF16)) for s in range(2)]
            yaT = [B(_sb(nc, "yaT%d" % s, [128, 8, 128], BF16)) for s in range(2)]
            sgwT = B(_sb(nc, "sgwT", [128, 8, 128], BF16))
            biasB = B(_sb(nc, "biasB", [128, 1024], F32))
            normB = B(_sb(nc, "normB", [128, 1024], F32))
            sgw = normB[:].rearrange("p (g q) -> p g q", g=8)
            sjunk = B(_sb(nc, "sjunk", [128, 1024], BF16))
            vst = B(_sb(nc, "vst", [128, NT, 6], F32))
            stmp = [B(_sb(nc, "stmp%d" % s, [128, 128], F32)) for s in range(2)]
            tmpr = [B(_sb(nc, "tmpr%d" % s, [128, 512], F32)) for s in range(2)]
            pss = [B(_ps(nc, "psc%d" % s, [128, 512], F32)) for s in range(8)]
            psi = [0]

            def nps():
                k = psi[0] % 8
                psi[0] += 1
                return ("psc", k), pss[k]
            P.dma("pool", wU[:], rows_view(G.ab_w_in[0, :, 0:1024]), writes=["wU"], semkey="wU")
            P.dma("pool", wV[:], rows_view(G.ab_w_in[0, :, 1024:2048]), writes=["wV"], semkey="wV")
            P.dma("pool", woA[:], rows_view(G.ab_w_out[0, 0:1024, :]), writes=["woA"], semkey="woA")
            P.dma("sp", sgw, G.sgu_w[0].rearrange("g p q -> p g q"), writes=["normB"], semkey="sgw")
            for g in range(8):
                pk, ps = nps()
                P.tr(ps[:, 0:128], sgw[:, g, :], G.ident[:], reads=["normB", "ident"], writes=[pk])
                P.dve(lambda e, g=g, ps=ps: e.tensor_copy(out=sgwT[:, g, :], in_=ps[:, 0:128]),
                      reads=[pk], writes=["sgwT"])
            row = gv[0:1, :]
            for (src, dst, nm) in ((G.sgu_b[0:1].rearrange("o g p -> o (g p)"), biasB, "biasB"),
                                   (G.sgu_norm[0:1, :], normB, "normB")):
                P.dma("sp", row, src, writes=[("gv", 0), ("gv", 1)], semkey="sgrow")
                for hb in range(2):
                    pk, ps = nps()
                    P.mm(ps[:, :], [(G.ones[0:1, 0:128], row[0:1, hb * 512:(hb + 1) * 512])],
                         reads=["ones", ("gv", 0), ("gv", 1)], writes=[pk])
                    P.act(lambda e, dst=dst, hb=hb, ps=ps: e.activation(out=dst[:, hb * 512:(hb + 1) * 512],
                                                                         in_=ps[:, :], func=AF.Identity),
                          reads=[pk], writes=[nm])
            ui = 0
            for bi, (a, n) in enumerate(TOKBLK):
                for g in range(8):
                    pk, ps = nps()
                    P.mm(ps[:, 0:n], [(wU[:, k, g * 128:(g + 1) * 128], hT[:, k, a:a + n]) for k in range(8)],
                         reads=["wU", "hTall"], writes=[pk])
                    P.act(lambda e, g=g, ps=ps, n=n: e.activation(out=guT[:, g, 0:n], in_=ps[:, 0:n],
                                                                  func=AF.Gelu_apprx_tanh),
                          reads=[pk], writes=[("guT", g)])
                for tt in range(n // 128):
                    t = a // 128 + tt
                    s = t % 2
                    for hb in range(2):
                        pk, ps = nps()
                        P.mm(ps[:, :], [(hT[:, k, t * 128:(t + 1) * 128], wV[:, k, hb * 512:(hb + 1) * 512])
                                        for k in range(8)], reads=["wV", "hTall"], writes=[pk])
                        P.act(lambda e, hb=hb, ps=ps: e.activation(out=gv[:, hb * 512:(hb + 1) * 512], in_=ps[:, :],
                                                                   func=AF.Gelu_apprx_tanh),
                              reads=[pk], writes=[("gv", hb)])
                    P.act(lambda e, t=t: e.activation(out=sjunk[:], in_=gv[:], func=AF.Square,
                                                      accum_out=vst[:, t, 0:1]),
                          reads=[("gv", 0), ("gv", 1)], writes=["sjunk", ("vst", t)])
                    P.dve(lambda e, t=t: e.tensor_scalar(out=vst[:, t, 1:2], in0=vst[:, t, 0:1], scalar1=1.0 / D,
                                                         scalar2=EPS, op0=ALU.mult, op1=ALU.add),
                          reads=[("vst", t)], writes=[("vst1", t)])
                    P.act(lambda e, t=t: e.activation(out=vst[:, t, 2:3], in_=vst[:, t, 1:2], func=AF.Sqrt),
                          reads=[("vst1", t)], writes=[("vst2", t)])
                    P.dve(lambda e, t=t: e.reciprocal(out=vst[:, t, 3:4], in_=vst[:, t, 2:3]),
                          reads=[("vst2", t)], writes=[("vst3", t)])
                    P.dve(lambda e, t=t, s=s: e.scalar_tensor_tensor(out=vc[s][:], in0=gv[:], scalar=vst[:, t, 3:4],
                                                                     in1=normB[:], op0=ALU.mult, op1=ALU.mult),
                          reads=[("gv", 0), ("gv", 1), ("vst3", t), "normB"], writes=[("vc", s)])
                    for g in range(8):
                        pk, ps = nps()
                        P.mm(ps[:, 0:128], [(vc[s][:, g * 128:(g + 1) * 128], sgwT[:, g, :])],
                             reads=[("vc", s), "sgwT"], writes=[pk])
                        ss = g % 2
                        P.dve(lambda e, ss=ss, ps=ps, g=g: e.tensor_tensor(
                            out=stmp[ss][:], in0=ps[:, 0:128], in1=biasB[:, g * 128:(g + 1) * 128], op=ALU.add),
                            reads=[pk, "biasB"], writes=[("stmp", ss)])
                        P.dve(lambda e, ss=ss, g=g, s=s, tt=tt: e.tensor_tensor(
                            out=yaT[s][:, g, :], in0=stmp[ss][:], in1=guT[:, g, tt * 128:(tt + 1) * 128], op=ALU.mult),
                            reads=[("stmp", ss), ("guT", g)], writes=[("yaT", s, g)])
                    for db in range(2):
                        pk, ps = nps()
                        P.mm(ps[:, :], [(yaT[s][:, k, :], woA[:, k, db * 512:(db + 1) * 512]) for k in range(8)],
                             reads=[("yaT", s, k) for k in range(8)] + ["woA"], writes=[pk])
                        resid_update(P, G, t, ps[:, :], pk, G.g1b, db * 512, 512, tmpr[ui % 2], ("tmpr", ui % 2))
                        ui += 1
            P.barrier()
            P.flush()


def phase_moe(P, G, i, ntiles):
    nc = P.nc
    with ExitStack() as ph:
        A = ph.enter_context
        tT = A(_sb(nc, "tT", [128, 8, T_ALL], BF16))
        gates = A(_sb(nc, "gates", [128, NT, 32], F32))
        with ExitStack() as p1:
            B = p1.enter_context
            scr = make_scr(P, B, 4, with_t32=True)
            wr = B(_sb(nc, "wr", [128, 8, 36], F32))
            br = B(_sb(nc, "br", [1, 36], F32))
            rt = [B(_sb(nc, "rt%d" % s, [128, 256], F32)) for s in range(2)]
            psr = [B(_ps(nc, "psr%d" % s, [128, 512], F32)) for s in range(2)]
            P.dma("sp", wr[:, :, 0:4], rows_view(G.moe_w_grp[i]), writes=["wr"], semkey="wr")
            P.dma("sp", wr[:, :, 4:36], rows_view(G.moe_w_exp[i]), writes=["wr"], semkey="wr")
            P.dma("sp", br[0:1, 0:4], G.moe_b_grp[i:i + 1, :], writes=["br"], semkey="br")
            P.dma("sp", br[0:1, 4:36], G.moe_b_exp[i:i + 1, :], writes=["br"], semkey="br")

            def router(t, s):
                R = rt[s]
                rk = ("rt", s)
                pk = ("psr", s)
                pairs = [(scr.t32[s][:, c, :], wr[:, c, :]) for c in range(8)]
                pairs.append((G.ones[0:1, 0:128], br[0:1, :]))
                P.mm(psr[s][:, 0:36], pairs, reads=[("t32", s, c) for c in range(8)] + ["wr", "br", "ones"],
                     writes=[pk])
                def d(fn, nm):
                    P.dve(fn, reads=[rk, pk], writes=[rk])
                d(lambda e: e.tensor_copy(out=R[:, 0:36], in_=psr[s][:, 0:36]), "lg")
                d(lambda e: e.reduce_max(out=R[:, 40:41], in_=R[:, 0:4], axis=mybir.AxisListType.X), "gmax")
                d(lambda e: e.tensor_scalar(out=R[:, 41:42], in0=R[:, 40:41], scalar1=-1.0, scalar2=None,
                                            op0=ALU.mult), "ngmax")
                P.act(lambda e: e.activation(out=R[:, 36:40], in_=R[:, 0:4], func=AF.Exp, bias=R[:, 41:42],
                                             accum_out=R[:, 42:43]), reads=[rk], writes=[rk])
                d(lambda e: e.reciprocal(out=R[:, 43:44], in_=R[:, 42:43]), "ptop")
                d(lambda e: e.tensor_scalar(out=R[:, 44:48], in0=R[:, 0:4], scalar1=R[:, 40:41], scalar2=None,
                                            op0=ALU.is_equal), "gmask")
                d(lambda e: e.tensor_scalar(out=R[:, 48:52], in0=R[:, 44:48], scalar1=BIG, scalar2=-BIG,
                                            op0=ALU.mult, op1=ALU.add), "pen")
                for g in range(4):
                    d(lambda e, g=g: e.tensor_scalar(out=R[:, 64 + 8 * g:72 + 8 * g], in0=R[:, 4 + 8 * g:12 + 8 * g],
                                                     scalar1=R[:, 48 + g:49 + g], scalar2=None, op0=ALU.add), "ml")
                d(lambda e: e.reduce_max(out=R[:, 52:53], in_=R[:, 64:96], axis=mybir.AxisListType.X), "m1")
                d(lambda e: e.tensor_scalar(out=R[:, 96:128], in0=R[:, 64:96], scalar1=R[:, 52:53], scalar2=None,
                                            op0=ALU.is_equal), "mask1")
                d(lambda e: e.scalar_tensor_tensor(out=R[:, 128:160], in0=R[:, 96:128], scalar=-BIG, in1=R[:, 64:96],
                                                   op0=ALU.mult, op1=ALU.add), "ml2")
                d(lambda e: e.reduce_max(out=R[:, 53:54], in_=R[:, 128:160], axis=mybir.AxisListType.X), "m2")
                d(lambda e: e.tensor_scalar(out=R[:, 160:192], in0=R[:, 128:160], scalar1=R[:, 53:54], scalar2=None,
                                            op0=ALU.is_equal), "mask2")
                d(lambda e: e.tensor_tensor(out=R[:, 54:55], in0=R[:, 53:54], in1=R[:, 52:53], op=ALU.subtract), "d")
                P.act(lambda e: e.activation(out=R[:, 55:56], in_=R[:, 54:55], func=AF.Exp), reads=[rk], writes=[rk])
                d(lambda e: e.tensor_scalar(out=R[:, 56:57], in0=R[:, 55:56], scalar1=1.0, scalar2=None,
                                            op0=ALU.add), "den")
                d(lambda e: e.reciprocal(out=R[:, 57:58], in_=R[:, 56:57]), "rden")
                d(lambda e: e.tensor_tensor(out=R[:, 58:59], in0=R[:, 57:58], in1=R[:, 43:44], op=ALU.mult), "w1")
                d(lambda e: e.tensor_tensor(out=R[:, 59:60], in0=R[:, 58:59], in1=R[:, 55:56], op=ALU.mult), "w2")
                d(lambda e: e.tensor_scalar(out=R[:, 192:224], in0=R[:, 96:128], scalar1=R[:, 58:59], scalar2=None,
                                            op0=ALU.mult), "t1")
                P.dve(lambda e: e.scalar_tensor_tensor(out=gates[:, t, :], in0=R[:, 160:192], scalar=R[:, 59:60],
                                                       in1=R[:, 192:224], op0=ALU.mult, op1=ALU.add),
                      reads=[rk], writes=["gates"])
            modulate(P, G, tT, 3, 4, ntiles, scr, router=router)
            P.barrier()
            P.flush()
        with ExitStack() as p2:
            B = p2.enter_context
            wg = [B(_sb(nc, "wg%d" % s, [128, 8, 512], BF16)) for s in range(2)]
            wu = [B(_sb(nc, "wu%d" % s, [128, 8, 512], BF16)) for s in range(2)]
            wd = [B(_sb(nc, "wd%d" % s, [128, 4, 1024], BF16)) for s in range(2)]
            he = [B(_sb(nc, "he%d" % s, [128, 4, 512], BF16)) for s in range(2)]
            sg = [B(_sb(nc, "sg%d" % s, [128, 512], F32)) for s in range(2)]
            tmpr = [B(_sb(nc, "tmpe%d" % s, [128, 512], F32)) for s in range(2)]
            psg = [B(_ps(nc, "psg%d" % s, [128, 512], F32)) for s in range(2)]
            psu = [B(_ps(nc, "psu%d" % s, [128, 512], F32)) for s in range(2)]
            psy = [B(_ps(nc, "psy%d" % s, [128, 512], F32)) for s in range(4)]
            blks = [b for b in TOKBLK if b[0] < ntiles * 128]
            steps = [(ex, bi) for ex in range(NEXP) for bi in range(len(blks))]
            cnt = {"ci": 0, "yi": 0}

            def emit_gu(i):
                ex, bi = steps[i]
                s = ex % 2
                if bi == 0:
                    P.dma("pool", wg[s][:], rows_view(G.moe_w_gate[i_layer, ex]), writes=[("wg", s)], semkey="wg%d" % s)
                    P.dma("pool", wu[s][:], rows_view(G.moe_w_up[i_layer, ex]), writes=[("wu", s)], semkey="wu%d" % s)
                    P.dma("pool", wd[s][:], rows_view(G.moe_w_down[i_layer, ex]), writes=[("wd", s)], semkey="wd%d" % s)
                (a, n) = blks[bi]
                hs = i % 2
                for m in range(4):
                    cs = cnt["ci"] % 2
                    cnt["ci"] += 1
                    P.mm(psg[cs][:, 0:n], [(wg[s][:, k, m * 128:(m + 1) * 128], tT[:, k, a:a + n]) for k in range(8)],
                         reads=[("wg", s), "tT"], writes=[("psg", cs)])
                    P.mm(psu[cs][:, 0:n], [(wu[s][:, k, m * 128:(m + 1) * 128], tT[:, k, a:a + n]) for k in range(8)],
                         reads=[("wu", s), "tT"], writes=[("psu", cs)])
                    P.act(lambda e, cs=cs, n=n: e.activation(out=sg[cs][:, 0:n], in_=psg[cs][:, 0:n], func=AF.Silu),
                          reads=[("psg", cs)], writes=[("sg", cs)])
                    P.dve(lambda e, cs=cs, n=n, hs=hs, m=m: e.tensor_tensor(
                        out=he[hs][:, m, 0:n], in0=sg[cs][:, 0:n], in1=psu[cs][:, 0:n], op=ALU.mult),
                        reads=[("sg", cs), ("psu", cs)], writes=[("he", hs, m)])

            def emit_down(i):
                ex, bi = steps[i]
                s = ex % 2
                (a, n) = blks[bi]
                hs = i % 2
                for tt in range(n // 128):
                    t = a // 128 + tt
                    for db in range(2):
                        ys = cnt["yi"] % 4
                        cnt["yi"] += 1
                        yi = cnt["yi"]
                        P.mm(psy[ys][:, :], [(he[hs][:, m, tt * 128:(tt + 1) * 128], wd[s][:, m, db * 512:(db + 1) * 512])
                                             for m in range(4)],
                             reads=[("he", hs, m) for m in range(4)] + [("wd", s)], writes=[("psy", ys)])
                        resid_update(P, G, t, psy[ys][:, :], ("psy", ys), G.g2b, db * 512, 512, tmpr[yi % 2],
                                     ("tmpe", yi % 2), gate=gates[:, t, ex:ex + 1])

            i_layer = i
            emit_gu(0)
            for si in range(len(steps)):
                if si + 1 < len(steps):
                    emit_gu(si + 1)
                emit_down(si)
            P.barrier()
            P.flush()


def phase_out(P, G):
    P.dma("sp", G.out.rearrange("(t p) d -> p t d", p=128), G.xl[:, 0:NT_L, :],
          reads=[("xl", t) for t in range(NT_L)], semkey="st")
    P.barrier()
    P.flush()


WEIGHT_NAMES = ["c_ctx", "ada_w", "ada_b", "ab_w_in", "sgu_norm", "sgu_w", "sgu_b", "conv_w", "ab_w_out",
                "cd_w_in", "s5_a_re", "s5_a_im", "s5_log_dt", "s5_b_re", "s5_b_im", "s5_c_re", "s5_c_im", "s5_d",
                "s5_w_glu", "mla_q_norm", "mla_kv_norm", "mla_w_uq", "mla_w_uk", "mla_w_uv", "mla_qn_gain",
                "mla_kn_gain", "cd_w_out", "moe_w_grp", "moe_b_grp", "moe_w_exp", "moe_b_exp", "moe_w_gate",
                "moe_w_up", "moe_w_down"]


def build(shapes, stages):
    nc = bass.Bass("TRN2", target_bir_lowering=False)
    G = SimpleNamespace()
    G.x = nc.dram_tensor("x", [SEQ, D], F32, kind="ExternalInput").ap()
    G.ctx = nc.dram_tensor("ctx", [CTX, D], F32, kind="ExternalInput").ap()
    G.c = nc.dram_tensor("c", [1, D], F32, kind="ExternalInput").ap()
    used = set(shapes.keys())
    for nm in WEIGHT_NAMES:
        if nm in used:
            shp = list(shapes[nm])
            if nm == "c_ctx":
                shp = [1, D]
            setattr(G, nm, nc.dram_tensor(nm, shp, F32, kind="ExternalInput").ap())
    G.out = nc.dram_tensor("out", [SEQ, D], F32, kind="ExternalOutput").ap()
    G.cd_parts = CD_PARTS
    if "mix1" in stages:
        G.ropC = nc.dram_tensor("ropC", [SEQ, 32], F32, kind="ExternalInput").ap()
        G.ropS = nc.dram_tensor("ropS", [SEQ, 32], F32, kind="ExternalInput").ap()
        G.zs = nc.dram_tensor("zs_scr", [12, 128, T_ALL], BF16, kind="Internal").ap()
        G.mixs = nc.dram_tensor("mix_scr", [8, 128, SEQ], BF16, kind="Internal").ap()
        G.qs = nc.dram_tensor("q_scr", [8, 96, SEQ], BF16, kind="Internal").ap()
        G.ks = nc.dram_tensor("k_scr", [8, 96, T_ALL], BF16, kind="Internal").ap()
        G.vs = nc.dram_tensor("v_scr", [NT, 128, 8, 65], BF16, kind="Internal").ap()
    with ExitStack() as st:
        E = st.enter_context
        P = Prog(nc, E)
        G.xl = E(_sb(nc, "xl", [128, NT, D], F32))
        G.g1b = E(_sb(nc, "gtile", [128, 2, D], F32))
        G.g2b = G.g1b
        G.modT = E(_sb(nc, "modT", [128, 6, 8, 2], F32))
        G.ident = E(_sb(nc, "ident", [128, 128], F32))
        G.iot = E(_sb(nc, "iot", [128, 128], I32))
        G.ones = E(_sb(nc, "ones", [128, 128], F32))
        G.onesb = E(_sb(nc, "onesb", [128, 128], BF16))
        phase_init(P, G)
        for stg in stages:
            if stg == "ada0":
                phase_ada(P, G, 0, [0, 1, 2, 3, 6, 7, 8, 9, 4, 5])
            elif stg == "mix0":
                phase_mixer_ab(P, G)
            elif stg == "moe0":
                phase_ada(P, G, 0, [10, 11])
                phase_moe(P, G, 0, NT)
            elif stg == "ada1":
                phase_ada(P, G, 1, [0, 1, 2, 3, 6, 7, 8, 9, 4, 5])
            elif stg == "mix1":
                phase_mixer_cd(P, G)
            elif stg == "moe1":
                phase_ada(P, G, 1, [10, 11])
                phase_moe(P, G, 1, NT_L)
        phase_out(P, G)
    return nc


ALL_STAGES = ["ada0", "mix0", "moe0", "ada1", "mix1", "moe1"]
CD_PARTS = ("s5", "mla")


def run(inputs, stages):
    needed = set(["c_ctx", "ada_w", "ada_b"])
    if "mix0" in stages:
        needed |= {"ab_w_in", "sgu_norm", "sgu_w", "sgu_b", "conv_w", "ab_w_out"}
    if "mix1" in stages:
        needed |= {"cd_w_in", "s5_a_re", "s5_a_im", "s5_log_dt", "s5_b_re", "s5_b_im", "s5_c_re", "s5_c_im", "s5_d",
                   "s5_w_glu", "mla_q_norm", "mla_kv_norm", "mla_w_uq", "mla_w_uk", "mla_w_uv", "mla_qn_gain",
                   "mla_kn_gain", "cd_w_out"}
    if "moe0" in stages or "moe1" in stages:
        needed |= {"moe_w_grp", "moe_b_grp", "moe_w_exp", "moe_b_exp", "moe_w_gate", "moe_w_up", "moe_w_down"}
    shapes = {k: inputs[k].shape for k in needed}
    nc = build(shapes, stages)
    shared = {}
    for k in needed:
        a = np.ascontiguousarray(np.asarray(inputs[k], dtype=np.float32))
        if k == "c_ctx":
            a = a.reshape(1, D)
        shared[k] = a
    if "mix1" in stages:
        pos = np.arange(SEQ)
        row = (pos // 64).astype(np.float32)
        colp = (pos % 64).astype(np.float32)
        inv = (np.float32(10000.0) ** (-np.arange(8, dtype=np.float32) / np.float32(8))).astype(np.float32)
        ar = (row[:, None] * inv[None, :]).astype(np.float32)
        ac = (colp[:, None] * inv[None, :]).astype(np.float32)
        cr, sr, cc, sc_ = np.cos(ar), np.sin(ar), np.cos(ac), np.sin(ac)
        shared["ropC"] = np.ascontiguousarray(np.concatenate([cr, cr, cc, cc], axis=1).astype(np.float32))
        shared["ropS"] = np.ascontiguousarray(np.concatenate([-sr, sr, -sc_, sc_], axis=1).astype(np.float32))
    x = np.asarray(inputs["x"], dtype=np.float32)
    ctx = np.asarray(inputs["ctx"], dtype=np.float32)
    c = np.asarray(inputs["c"], dtype=np.float32)
    in_maps = []
    for b in range(8):
        m = dict(shared)
        m["x"] = np.ascontiguousarray(x[b])
        m["ctx"] = np.ascontiguousarray(ctx[b])
        m["c"] = np.ascontiguousarray(c[b].reshape(1, D))
        in_maps.append(m)
    res = run_bass_kernel_spmd(nc, in_maps, core_ids=list(range(8)))
    return np.stack([np.asarray(r["out"], dtype=np.float32) for r in res.results], axis=0)


def kernel(**inputs):
    return run(inputs, ALL_STAGES)


def _rot(lst, name):
    st = [0]

    def f():
        k = st[0] % len(lst)
        st[0] += 1
        return (name, k), lst[k]
    return f


S5OPT = {"pool_rr": True, "pool_rotin": False, "prefetch": True}
TWO_PI = 2.0 * math.pi
PI_SAFE = 3.1415925


def Prog_mm1(P, out, lhsT, rhs, start, stop, reads=(), writes=()):
    P.op("pe", lambda e: e.matmul(out, lhsT, rhs, start=start, stop=stop), reads, writes)


def phase_mixer_cd(P, G):
    nc = P.nc
    X = mybir.AxisListType.X
    with ExitStack() as ph:
        A = ph.enter_context
        mst = A(_sb(nc, "mst", [128, NT, 40], F32))
        with ExitStack() as p1:
            B = p1.enter_context
            hT = B(_sb(nc, "hT1", [128, 8, T_ALL], BF16))
            with ExitStack() as p0:
                scr = make_scr(P, p0.enter_context, 4)
                modulate(P, G, hT, 0, 1, NT, scr)
                P.barrier()
                P.flush()
            wI = B(_sb(nc, "wI", [128, 8, 1568], BF16))
            stg = [B(_sb(nc, "zst%d" % s, [128, 512], BF16)) for s in range(4)]
            sqj = B(_sb(nc, "sqj", [128, 512], BF16))
            nps = _rot([B(_ps(nc, "psd%d" % s, [128, 512], F32)) for s in range(8)], "psd")
            P.dma("pool", wI[:], rows_view(G.cd_w_in[0]), writes=["wI"], semkey="wI")
            si = 0
            for j in range(12):
                for (a, n) in TOKBLK:
                    pk, ps = nps()
                    P.mm(ps[:, 0:n], [(wI[:, k, j * 128:(j + 1) * 128], hT[:, k, a:a + n]) for k in range(8)],
                         reads=["wI"], writes=[pk])
                    s = si % 4
                    si += 1
                    if si % 2:
                        P.act(lambda e, s=s, ps=ps, n=n: e.activation(out=stg[s][:, 0:n], in_=ps[:, 0:n], func=AF.Identity),
                              reads=[pk], writes=[("zst", s)])
                    else:
                        P.dve(lambda e, s=s, ps=ps, n=n: e.tensor_copy(out=stg[s][:, 0:n], in_=ps[:, 0:n]),
                              reads=[pk], writes=[("zst", s)])
                    P.dma("sp", G.zs[j, :, a:a + n], stg[s][:, 0:n], reads=[("zst", s)], writes=[("zs", j)],
                          semkey="zst%d" % s)
            for t in range(NT):
                hs = [hT[:, k, t * 128:(t + 1) * 128] for k in range(8)]
                pk, ps = nps()
                P.mm(ps[:, :], [(hs[k], wI[:, k, 512:1024]) for k in range(8)], reads=["wI"], writes=[pk])
                P.act(lambda e, ps=ps, t=t: e.activation(out=sqj[:, :], in_=ps[:, :], func=AF.Square,
                                                         accum_out=mst[:, t, 0:1]), reads=[pk], writes=["sqj", "mst"])
                pk, ps = nps()
                P.mm(ps[:, :], [(hs[k], wI[:, k, 1024:1536]) for k in range(8)], reads=["wI"], writes=[pk])
                P.act(lambda e, ps=ps, t=t: e.activation(out=sqj[:, 0:256], in_=ps[:, 0:256], func=AF.Square,
                                                         accum_out=mst[:, t, 1:2]), reads=[pk], writes=["sqj", "mst"])
                P.act(lambda e, ps=ps, t=t: e.activation(out=sqj[:, 256:512], in_=ps[:, 256:512], func=AF.Square,
                                                         accum_out=mst[:, t, 2:3]), reads=[pk], writes=["sqj", "mst"])
                pk, ps = nps()
                P.mm(ps[:, 0:32], [(hs[k], wI[:, k, 1536:1568]) for k in range(8)], reads=["wI"], writes=[pk])
                P.dve(lambda e, ps=ps, t=t: e.tensor_copy(out=mst[:, t, 8:40], in_=ps[:, 0:32]),
                      reads=[pk], writes=["mst"])
            m = ["mst"]
            P.dve(lambda e: e.tensor_tensor(out=mst[:, :, 5], in0=mst[:, :, 0], in1=mst[:, :, 1], op=ALU.add), m, m)
            P.dve(lambda e: e.tensor_scalar(out=mst[:, :, 5], in0=mst[:, :, 5], scalar1=1.0 / 768, scalar2=EPS,
                                            op0=ALU.mult, op1=ALU.add), m, m)
            P.act(lambda e: e.activation(out=mst[:, :, 6], in_=mst[:, :, 5], func=AF.Sqrt), m, m)
            P.dve(lambda e: e.reciprocal(out=mst[:, :, 3], in_=mst[:, :, 6]), m, m)
            P.dve(lambda e: e.tensor_scalar(out=mst[:, :, 5], in0=mst[:, :, 2], scalar1=1.0 / 256, scalar2=EPS,
                                            op0=ALU.mult, op1=ALU.add), m, m)
            P.act(lambda e: e.activation(out=mst[:, :, 6], in_=mst[:, :, 5], func=AF.Sqrt), m, m)
            P.dve(lambda e: e.reciprocal(out=mst[:, :, 4], in_=mst[:, :, 6]), m, m)
            P.barrier()
            P.flush()
        if "s5" in G.cd_parts:
            _cd_s5(P, G, nc)
        if "mla" in G.cd_parts:
            _cd_mla(P, G, nc, mst)
        with ExitStack() as p4:
            B = p4.enter_context
            mixT = B(_sb(nc, "mixT", [128, 8, SEQ], BF16))
            woC = B(_sb(nc, "woC", [128, 8, 1024], BF16))
            tmpr = [B(_sb(nc, "tmpo%d" % s, [128, 512], F32)) for s in range(2)]
            nps = _rot([B(_ps(nc, "pso%d" % s, [128, 512], F32)) for s in range(4)], "pso")
            ks = []
            if "s5" in G.cd_parts:
                ks += [0, 1, 2, 3]
            if "mla" in G.cd_parts:
                ks += [4, 5, 6, 7]
            for j in ks:
                P.dma("sp", mixT[:, j, :], G.mixs[j], writes=[("mixT", j)], semkey="mixT%d" % j)
            P.dma("pool", woC[:], rows_view(G.cd_w_out[0]), writes=["woC"], semkey="woC")
            ui = 0
            for t in range(NT_L):
                for db in range(2):
                    pk, ps = nps()
                    P.mm(ps[:, :], [(mixT[:, k, t * 128:(t + 1) * 128], woC[:, k, db * 512:(db + 1) * 512]) for k in ks],
                         reads=[("mixT", k) for k in ks] + ["woC"], writes=[pk])
                    resid_update(P, G, t, ps[:, :], pk, G.g1b, db * 512, 512, tmpr[ui % 2], ("tmpo", ui % 2))
                    ui += 1
            P.barrier()
            P.flush()


def _cd_s5(P, G, nc):
    T = T_ALL
    with ExitStack() as p2:
        Bp = p2.enter_context
        V = Bp(_sb(nc, "s5v", [128, 24, 32], F32))
        VI = Bp(_sb(nc, "s5vi", [128, 32], I32))
        hpi = Bp(_sb(nc, "hpi", [128, 1], F32))
        dsk = Bp(_sb(nc, "dsk", [128, 4], F32))
        ldrow = Bp(_sb(nc, "ldrow", [1, 64], F32))
        gT = Bp(_sb(nc, "s5g", [128, 4, SEQ], BF16))
        AR, AI, LDT, DT, TMP, MAG, TH, THR, SN, AB, CS, ABR, ABI, DEN, RDEN, NR, FRE, FIM, NFIM, T2, T3 = range(21)
        with ExitStack() as pa:
            B = pa.enter_context
            NF = B(_sb(nc, "s5nf", [128, T], F32))
            TSb = [B(_sb(nc, "s5ts%d" % s, [128, T], F32)) for s in range(2)]
            TCb = [B(_sb(nc, "s5tc%d" % s, [128, T], F32)) for s in range(2)]
            GR = B(_sb(nc, "s5gr", [128, T], F32))
            GI = B(_sb(nc, "s5gi", [128, T], F32))
            KI = GI[:].bitcast(I32)
            HRb = [B(_sb(nc, "s5hr%d" % s, [128, 512], BF16)) for s in range(2)]
            HIb = [B(_sb(nc, "s5hi%d" % s, [128, 512], BF16)) for s in range(2)]
            tmp = [B(_sb(nc, "s5tmp%d" % s, [128, 512], F32)) for s in range(4)]
            uTc = B(_sb(nc, "s5u", [128, T], BF16))
            yacc = B(_sb(nc, "s5y", [128, SEQ], F32))
            XRE = [B(_sb(nc, "xre%d" % s, [128, 128], F32)) for s in range(2)]
            XIM = [B(_sb(nc, "xim%d" % s, [128, 128], F32)) for s in range(2)]
            BT = [B(_sb(nc, "bt%d" % s, [128, 128], F32)) for s in range(2)]
            BB = [B(_sb(nc, "bb%d" % s, [128, 2, 128], F32)) for s in range(2)]
            LB = [B(_sb(nc, "lb%d" % s, [128, 2, 128], BF16)) for s in range(2)]
            LC = [B(_sb(nc, "lc%d" % s, [128, 2, 128], BF16)) for s in range(2)]
            YY = [B(_sb(nc, "yy%d" % s, [32, 2, 128], F32)) for s in range(2)]
            nps = _rot([B(_ps(nc, "pss%d" % s, [128, 512], F32)) for s in range(8)], "pss")
            v = ["s5v"]
            for (src, slot) in ((G.s5_a_re, 0), (G.s5_a_im, 1)):
                for g2 in range(2):
                    P.dma("sp", V[g2 * 64:(g2 + 1) * 64, slot, :].rearrange("p (d q) -> p d q", d=2),
                          src[0].rearrange("d (q g) p -> g p d q", g=2)[g2], writes=v, semkey="s5v",
                          allow_slow_non_contiguous=True)
            P.dma("sp", ldrow[0:1, :], G.s5_log_dt[0:1].rearrange("o d g -> o (d g)"), writes=["ldrow"], semkey="ldrow")
            P.dma("sp", dsk[:, :], G.s5_d[0].rearrange("(k p) -> p k", p=128), writes=["dsk"], semkey="dsk",
                  allow_slow_non_contiguous=True)
            P.pool(lambda e: e.memset(hpi[:], math.pi / 2), writes=["hpi"])
            P.pool(lambda e: e.iota(NF[:], [[1, T]], base=1, channel_multiplier=0, allow_small_or_imprecise_dtypes=True),
                   writes=["NF"])
            pk, ps = nps()
            P.mm(ps[:, 0:64], [(G.ones[0:1, 0:128], ldrow[0:1, :])], reads=["ldrow"], writes=[pk])
            psv = ps[:, 0:64].rearrange("p (d q g) -> p d q g", d=2, g=2)
            P.dve(lambda e: e.tensor_copy(out=V[0:64, 2, :].rearrange("p (d q) -> p d q", d=2), in_=psv[0:64, :, :, 0]),
                  reads=[pk], writes=v)
            P.dve(lambda e: e.tensor_copy(out=V[64:128, 2, :].rearrange("p (d q) -> p d q", d=2), in_=psv[64:128, :, :, 1]),
                  reads=[pk], writes=v)

            def vv(i):
                return V[:, i, :]

            def tt(o, a, b, op):
                P.dve(lambda e: e.tensor_tensor(out=vv(o), in0=vv(a), in1=vv(b), op=op), v, v)
            P.act(lambda e: e.activation(out=vv(DT), in_=vv(LDT), func=AF.Exp), v, v)
            tt(TMP, DT, AR, ALU.mult)
            P.act(lambda e: e.activation(out=vv(MAG), in_=vv(TMP), func=AF.Exp), v, v)
            tt(TH, DT, AI, ALU.mult)
            P.dve(lambda e: e.tensor_scalar(out=VI[:, :], in0=vv(TH), scalar1=1.0 / TWO_PI, scalar2=None, op0=ALU.mult), v, v)
            P.dve(lambda e: e.scalar_tensor_tensor(out=vv(THR), in0=VI[:, :], scalar=-TWO_PI, in1=vv(TH), op0=ALU.mult,
                                                   op1=ALU.add), v, v)
            P.dve(lambda e: e.tensor_scalar(out=vv(THR), in0=vv(THR), scalar1=-PI_SAFE, scalar2=PI_SAFE, op0=ALU.max,
                                            op1=ALU.min), v, v)
            P.act(lambda e: e.activation(out=vv(SN), in_=vv(THR), func=AF.Sin), v, v)
            P.act(lambda e: e.activation(out=vv(AB), in_=vv(THR), func=AF.Abs), v, v)
            P.act(lambda e: e.activation(out=vv(CS), in_=vv(AB), func=AF.Sin, scale=-1.0, bias=hpi[:, 0:1]),
                  v + ["hpi"], v)
            tt(ABR, MAG, CS, ALU.mult)
            tt(ABI, MAG, SN, ALU.mult)
            tt(DEN, AR, AR, ALU.mult)
            tt(T2, AI, AI, ALU.mult)
            tt(DEN, DEN, T2, ALU.add)
            P.dve(lambda e: e.reciprocal(out=vv(RDEN), in_=vv(DEN)), v, v)
            P.dve(lambda e: e.tensor_scalar(out=vv(NR), in0=vv(ABR), scalar1=-1.0, scalar2=None, op0=ALU.add), v, v)
            tt(T2, NR, AR, ALU.mult)
            tt(T3, ABI, AI, ALU.mult)
            tt(T2, T2, T3, ALU.add)
            tt(FRE, T2, RDEN, ALU.mult)
            tt(T2, ABI, AR, ALU.mult)
            tt(T3, NR, AI, ALU.mult)
            tt(T2, T2, T3, ALU.subtract)
            tt(FIM, T2, RDEN, ALU.mult)
            P.dve(lambda e: e.tensor_scalar(out=vv(NFIM), in0=vv(FIM), scalar1=-1.0, scalar2=None, op0=ALU.mult), v, v)
            P.barrier()
            vr = []

            iters = [(ch, pic, d) for ch in range(4) for pic in range(4) for d in range(2)]

            def prep(it):
                ch, pic, d = iters[it]
                s = it % 2
                q = ch * 4 + pic
                col = d * 16 + q
                c0 = 32 * pic
                xk, bk, lbk, lck, yk = ("X", s), ("BB", s), ("LB", s), ("LC", s), ("YY", s)
                TS, TC = TSb[s], TCb[s]
                tsk, tck = ("TS", s), ("TC", s)
                P.pool(lambda e: e.memset(XRE[s][:], 0.0), writes=[xk])
                P.pool(lambda e: e.memset(XIM[s][:], 0.0), writes=[xk])
                P.pool(lambda e: e.memset(YY[s][:], 0.0), writes=[yk])
                P.pool(lambda e: e.memset(LC[s][:], 0.0), writes=[lck])
                for g2 in range(2):
                    g = 2 * q + g2
                    cc = c0 + 16 * g2
                    P.dma("sp", XRE[s][g2 * 64:(g2 + 1) * 64, cc:cc + 16], G.s5_b_re[0, d, g], writes=[xk],
                          semkey="X%d" % s)
                    P.dma("sp", XIM[s][g2 * 64:(g2 + 1) * 64, cc:cc + 16], G.s5_b_im[0, d, g], writes=[xk],
                          semkey="X%d" % s)
                    P.dma("sp", YY[s][g2 * 16:(g2 + 1) * 16, 0, g2 * 64:(g2 + 1) * 64], G.s5_c_re[0, d, g],
                          writes=[yk], semkey="Y%d" % s)
                    P.dma("sp", YY[s][g2 * 16:(g2 + 1) * 16, 1, g2 * 64:(g2 + 1) * 64], G.s5_c_im[0, d, g],
                          writes=[yk], semkey="Y%d" % s)
                fre, fim, nfim = V[:, FRE, col:col + 1], V[:, FIM, col:col + 1], V[:, NFIM, col:col + 1]
                P.dve(lambda e: e.tensor_scalar(out=BT[s][:], in0=XRE[s][:], scalar1=fre, scalar2=None, op0=ALU.mult),
                      reads=[xk], writes=[("BT", s)])
                P.dve(lambda e: e.scalar_tensor_tensor(out=BB[s][:, 0, :], in0=XIM[s][:], scalar=nfim, in1=BT[s][:],
                                                       op0=ALU.mult, op1=ALU.add), reads=[xk, ("BT", s)], writes=[bk])
                P.dve(lambda e: e.tensor_scalar(out=BT[s][:], in0=XIM[s][:], scalar1=fre, scalar2=None, op0=ALU.mult),
                      reads=[xk], writes=[("BT", s)])
                P.dve(lambda e: e.scalar_tensor_tensor(out=BB[s][:, 1, :], in0=XRE[s][:], scalar=fim, in1=BT[s][:],
                                                       op0=ALU.mult, op1=ALU.add), reads=[xk, ("BT", s)], writes=[bk])
                for ri in range(2):
                    pk, ps = nps()
                    P.tr(ps[:, 0:128], BB[s][:, ri, :], G.ident[:], reads=[bk], writes=[pk])
                    P.act(lambda e, ri=ri, ps=ps: e.activation(out=LB[s][:, ri, :], in_=ps[:, 0:128], func=AF.Identity),
                          reads=[pk], writes=[lbk])
                    pk, ps = nps()
                    P.tr(ps[:, 0:32], YY[s][0:32, ri, :], G.ident[0:32, 0:32], reads=[yk], writes=[pk])
                    P.act(lambda e, ri=ri, ps=ps: e.activation(out=LC[s][:, ri, c0:c0 + 32], in_=ps[:, 0:32],
                                                                func=AF.Identity, scale=(1.0 if ri == 0 else -1.0)),
                          reads=[pk], writes=[lck])
                thr = V[:, THR, col:col + 1]
                P.act(lambda e: e.activation(out=TS[:, :], in_=NF[:, :], func=AF.Identity, scale=thr),
                      reads=["NF"], writes=[tsk])
                KT = TC[:].bitcast(I32)
                rr = P.pool if S5OPT["pool_rr"] else P.dve
                rr(lambda e: e.tensor_scalar(out=KT, in0=TS[:, :], scalar1=1.0 / TWO_PI, scalar2=0.0, op0=ALU.mult,
                                             op1=ALU.add), reads=[tsk], writes=[tck])
                P.dve(lambda e: e.scalar_tensor_tensor(out=TS[:, :], in0=KT, scalar=-TWO_PI, in1=TS[:, :], op0=ALU.mult,
                                                       op1=ALU.add), reads=[tck, tsk], writes=[tsk])
                rr(lambda e: e.tensor_scalar(out=TS[:, :], in0=TS[:, :], scalar1=PI_SAFE, scalar2=-PI_SAFE,
                                             op0=ALU.min, op1=ALU.max), reads=[tsk], writes=[tsk])
                P.act(lambda e: e.activation(out=TC[:, :], in_=TS[:, :], func=AF.Abs), reads=[tsk, tck], writes=[tck])
                P.act(lambda e: e.activation(out=TS[:, :], in_=TS[:, :], func=AF.Sin), reads=[tsk, tck], writes=[tsk])
                P.act(lambda e: e.activation(out=TC[:, :], in_=TC[:, :], func=AF.Sin, scale=-1.0, bias=hpi[:, 0:1]),
                      reads=[tck, "hpi"], writes=[tck])

            if S5OPT["prefetch"]:
                prep(0)
            hb_i = 0
            for it, (ch, pic, d) in enumerate(iters):
                if not S5OPT["prefetch"]:
                    prep(it)
                s = it % 2
                q = ch * 4 + pic
                col = d * 16 + q
                lbk, lck = ("LB", s), ("LC", s)
                TS, TC = TSb[s], TCb[s]
                tsk, tck = ("TS", s), ("TC", s)
                if pic == 0 and d == 0:
                    P.dma("sp", uTc[:, :], G.zs[ch], writes=["uTc"], semkey="uTc")
                    P.dve(lambda e, ch=ch: e.tensor_scalar(out=yacc[:, :], in0=uTc[:, 0:SEQ], scalar1=dsk[:, ch:ch + 1],
                                                           scalar2=None, op0=ALU.mult),
                          reads=["uTc", "dsk"], writes=["yacc"])

                def tsl(tbl, b0, n, d=d):
                    if d == 0:
                        return tbl[:, b0:b0 + n]
                    return tbl[:, T - b0 - n:T - b0][:, ::-1]
                for bi, (a, n) in enumerate(TOKBLK):
                    if d == 0:
                        b0 = a + CTX if a < SEQ else a - SEQ
                    else:
                        b0 = a
                    pkr, psr = nps()
                    P.mm(psr[:, 0:n], [(LB[s][:, 0, :], uTc[:, a:a + n])], reads=[lbk, "uTc"], writes=[pkr])
                    pki, psi = nps()
                    P.mm(psi[:, 0:n], [(LB[s][:, 1, :], uTc[:, a:a + n])], reads=[lbk, "uTc"], writes=[pki])
                    tc_, ts_ = tsl(TC, b0, n), tsl(TS, b0, n)
                    ta, tb, tc2, td2 = tmp[0], tmp[1], tmp[2], tmp[3]
                    ka, kb, kc2, kd2 = ("tmp", 0), ("tmp", 1), ("tmp", 2), ("tmp", 3)
                    gr, gi = GR[:, b0:b0 + n], GI[:, b0:b0 + n]
                    P.act(lambda e, ta=ta, psr=psr, n=n: e.activation(out=ta[:, 0:n], in_=psr[:, 0:n], func=AF.Identity),
                          reads=[pkr], writes=[ka])
                    P.act(lambda e, tb=tb, psi=psi, n=n: e.activation(out=tb[:, 0:n], in_=psi[:, 0:n], func=AF.Identity),
                          reads=[pki], writes=[kb])
                    ri_ = P.pool if S5OPT["pool_rotin"] else P.dve
                    ri_(lambda e, ta=ta, tc2=tc2, ts_=ts_, n=n: e.tensor_tensor(out=tc2[:, 0:n], in0=ta[:, 0:n], in1=ts_,
                                                                                 op=ALU.mult),
                        reads=[ka, tsk], writes=[kc2])
                    ri_(lambda e, tb=tb, td2=td2, ts_=ts_, n=n: e.tensor_tensor(out=td2[:, 0:n], in0=tb[:, 0:n], in1=ts_,
                                                                                 op=ALU.mult),
                        reads=[kb, tsk], writes=[kd2])
                    P.dve(lambda e, gr=gr, psr=psr, tc_=tc_, n=n: e.tensor_tensor(out=gr, in0=psr[:, 0:n], in1=tc_,
                                                                                   op=ALU.mult),
                          reads=[pkr, tck], writes=["GR"])
                    P.dve(lambda e, gi=gi, psi=psi, tc_=tc_, n=n: e.tensor_tensor(out=gi, in0=psi[:, 0:n], in1=tc_,
                                                                                   op=ALU.mult),
                          reads=[pki, tck], writes=["GI"])
                    P.dve(lambda e, gr=gr, td2=td2, n=n: e.tensor_tensor(out=gr, in0=gr, in1=td2[:, 0:n], op=ALU.add),
                          reads=[kd2, "GR"], writes=["GR"])
                    P.dve(lambda e, gi=gi, tc2=tc2, n=n: e.tensor_tensor(out=gi, in0=gi, in1=tc2[:, 0:n], op=ALU.subtract),
                          reads=[kc2, "GI"], writes=["GI"])
                magb = V[:, MAG, col:col + 1].to_broadcast([128, T])
                for (buf, nm) in ((GR, "GR"), (GI, "GI")):
                    bv = buf[:, :] if d == 0 else buf[:, ::-1]
                    P.dve(lambda e, bv=bv, magb=magb: e.tensor_tensor_scan(out=bv, data0=magb, data1=bv, initial=0.0,
                                                                           op0=ALU.mult, op1=ALU.add),
                          reads=[nm], writes=[nm])
                if S5OPT["prefetch"] and it + 1 < len(iters):
                    prep(it + 1)
                for bi in range(4):
                    a = bi * 512
                    b0 = a + CTX if d == 0 else a
                    n = 512
                    tc_, ts_ = tsl(TC, b0, n), tsl(TS, b0, n)
                    gr, gi = GR[:, b0:b0 + n], GI[:, b0:b0 + n]
                    tk = [("tmp", i) for i in range(4)]
                    hb = hb_i % 2
                    hb_i += 1
                    P.dve(lambda e, gr=gr, tc_=tc_: e.tensor_tensor(out=tmp[0][:], in0=gr, in1=tc_, op=ALU.mult),
                          reads=["GR", tck], writes=[tk[0]])
                    P.pool(lambda e, gi=gi, ts_=ts_: e.tensor_tensor(out=tmp[1][:], in0=gi, in1=ts_, op=ALU.mult),
                           reads=["GI", tsk], writes=[tk[1]])
                    P.pool(lambda e, gr=gr, ts_=ts_: e.tensor_tensor(out=tmp[2][:], in0=gr, in1=ts_, op=ALU.mult),
                           reads=["GR", tsk], writes=[tk[2]])
                    P.dve(lambda e, gi=gi, tc_=tc_: e.tensor_tensor(out=tmp[3][:], in0=gi, in1=tc_, op=ALU.mult),
                          reads=["GI", tck], writes=[tk[3]])
                    P.dve(lambda e, hb=hb: e.tensor_tensor(out=HRb[hb][:], in0=tmp[0][:], in1=tmp[1][:],
                                                           op=ALU.subtract), reads=[tk[0], tk[1]], writes=[("HR", hb)])
                    P.dve(lambda e, hb=hb: e.tensor_tensor(out=HIb[hb][:], in0=tmp[2][:], in1=tmp[3][:], op=ALU.add),
                          reads=[tk[2], tk[3]], writes=[("HI", hb)])
                    pk, ps = nps()
                    P.mm(ps[:, :], [(LC[s][:, 0, :], HRb[hb][:]), (LC[s][:, 1, :], HIb[hb][:])],
                         reads=[lck, ("HR", hb), ("HI", hb)], writes=[pk])
                    P.dve(lambda e, a=a, ps=ps: e.tensor_tensor(out=yacc[:, a:a + 512], in0=yacc[:, a:a + 512],
                                                                in1=ps[:, :], op=ALU.add),
                          reads=[pk, "yacc"], writes=["yacc"])
                if pic == 3 and d == 1:
                    P.act(lambda e, ch=ch: e.activation(out=gT[:, ch, :], in_=yacc[:, :], func=AF.Gelu_apprx_tanh),
                          reads=["yacc"], writes=[("gT", ch)])
            P.barrier()
            P.flush()
        with ExitStack() as pb:
            B = pb.enter_context
            wgl = B(_sb(nc, "wglu", [128, 4, 512], BF16))
            sig = [B(_sb(nc, "s5sig%d" % s, [128, 512], F32)) for s in range(2)]
            sst = [B(_sb(nc, "s5st%d" % s, [128, 512], BF16)) for s in range(2)]
            nps = _rot([B(_ps(nc, "psg%d" % s, [128, 512], F32)) for s in range(4)], "psg")
            P.dma("pool", wgl[:], rows_view(G.s5_w_glu[0]), writes=["wglu"], semkey="wglu")
            gi_ = 0
            for m in range(4):
                for bi in range(4):
                    a = bi * 512
                    pk, ps = nps()
                    P.mm(ps[:, :], [(wgl[:, k, m * 128:(m + 1) * 128], gT[:, k, a:a + 512]) for k in range(4)],
                         reads=["wglu"], writes=[pk])
                    s = gi_ % 2
                    gi_ += 1
                    P.act(lambda e, s=s, ps=ps: e.activation(out=sig[s][:], in_=ps[:, :], func=AF.Sigmoid),
                          reads=[pk], writes=[("sig", s)])
                    P.dve(lambda e, s=s, m=m, a=a: e.tensor_tensor(out=sst[s][:], in0=sig[s][:], in1=gT[:, m, a:a + 512],
                                                                   op=ALU.mult),
                          reads=[("sig", s)], writes=[("sst", s)])
                    P.dma("sp", G.mixs[m, :, a:a + 512], sst[s][:], reads=[("sst", s)], writes=[("mixs", m)],
                          semkey="sst%d" % s)
            P.barrier()
            P.flush()


def _cd_mla(P, G, nc, mst):
    AX = mybir.AxisListType.X
    QSCALE = 96.0 ** -0.5
    with ExitStack() as c1:
        C = c1.enter_context
        wq = C(_sb(nc, "wq", [128, 6, 768], BF16))
        wk = C(_sb(nc, "wk", [128, 2, 512], BF16))
        wv = C(_sb(nc, "wv", [128, 2, 512], BF16))
        wst = [C(_sb(nc, "wst%d" % s, [128, 768], F32)) for s in range(2)]
        nT = C(_sb(nc, "nT", [128, 8], F32))
        grow = C(_sb(nc, "grow", [1, 192], F32))
        GQ = C(_sb(nc, "GQ", [128, 96], F32))
        GK = C(_sb(nc, "GK", [128, 96], F32))
        ropC = C(_sb(nc, "ropC", [128, NT_L, 32], F32))
        ropS = C(_sb(nc, "ropS", [128, NT_L, 32], F32))
        identb = C(_sb(nc, "identb", [128, 128], BF16))
        cqt = [C(_sb(nc, "cqt%d" % s, [128, 6, 128], BF16)) for s in range(2)]
        ckt = [C(_sb(nc, "ckt%d" % s, [128, 2, 128], BF16)) for s in range(2)]
        qf = [C(_sb(nc, "qf%d" % s, [128, 8, 96], F32)) for s in range(2)]
        kf = [C(_sb(nc, "kf%d" % s, [128, 8, 96], F32)) for s in range(2)]
        sq = [C(_sb(nc, "sqh%d" % s, [128, 8, 96], F32)) for s in range(4)]
        hs = [C(_sb(nc, "hs%d" % s, [128, 4, 8], F32)) for s in range(4)]
        R1 = [C(_sb(nc, "R1_%d" % s, [128, 8, 32], F32)) for s in range(4)]
        R2 = [C(_sb(nc, "R2_%d" % s, [128, 8, 32], F32)) for s in range(4)]
        xb = [C(_sb(nc, "xb%d" % s, [128, 8, 96], BF16)) for s in range(4)]
        xst = [C(_sb(nc, "xst%d" % s, [128, 8, 128], BF16)) for s in range(4)]
        vst = [C(_sb(nc, "vst%d" % s, [128, 8, 65], BF16)) for s in range(2)]
        psf = [C(_ps(nc, "psm%d" % s, [128, 512], F32)) for s in range(5)]
        ptb = [C(_ps(nc, "pst%d" % s, [128, 1024], BF16)) for s in range(2)]
        nps = _rot([psf[4]], "psm4")
        P.dma("sp", nT[:, 0:6], G.mla_q_norm[0].rearrange("(k p) -> p k", p=128), writes=["nT"], semkey="nT",
              allow_slow_non_contiguous=True)
        P.dma("sp", nT[:, 6:8], G.mla_kv_norm[0].rearrange("(k p) -> p k", p=128), writes=["nT"], semkey="nT",
              allow_slow_non_contiguous=True)
        wi = 0
        jobs = [(G.mla_w_uq[0, k * 128:(k + 1) * 128, :], wq[:, k, :], 768, k) for k in range(6)]
        jobs += [(G.mla_w_uk[0, k * 128:(k + 1) * 128, :], wk[:, k, :], 512, 6 + k) for k in range(2)]
        jobs += [(G.mla_w_uv[0, k * 128:(k + 1) * 128, :], wv[:, k, :], 512, 6 + k) for k in range(2)]
        for (src, dst, n, nc_) in jobs:
            s = wi % 2
            wi += 1
            P.dma("sp", wst[s][:, 0:n], src, writes=[("wst", s)], semkey="wst%d" % s)
            P.dve(lambda e, s=s, dst=dst, n=n, nc_=nc_: e.tensor_scalar(out=dst, in0=wst[s][:, 0:n],
                                                                         scalar1=nT[:, nc_:nc_ + 1], scalar2=None,
                                                                         op0=ALU.mult),
                  reads=[("wst", s), "nT"], writes=["wqkv"])
        P.dma("sp", grow[0:1, 0:96], G.mla_qn_gain[0:1, :], writes=["grow"], semkey="grow")
        P.dma("sp", grow[0:1, 96:192], G.mla_kn_gain[0:1, :], writes=["grow"], semkey="grow")
        pk, ps = nps()
        P.mm(ps[:, 0:192], [(G.ones[0:1, 0:128], grow[0:1, :])], reads=["grow"], writes=[pk])
        P.act(lambda e, ps=ps: e.activation(out=GQ[:, :], in_=ps[:, 0:96], func=AF.Identity, scale=QSCALE),
              reads=[pk], writes=["GQ"])
        P.act(lambda e, ps=ps: e.activation(out=GK[:, :], in_=ps[:, 96:192], func=AF.Identity),
              reads=[pk], writes=["GK"])
        P.dma("sp", ropC[:], G.ropC.rearrange("(t p) f -> p t f", p=128), writes=["rop"], semkey="ropC")
        P.dma("sp", ropS[:], G.ropS.rearrange("(t p) f -> p t f", p=128), writes=["rop"], semkey="ropS")
        P.dve(lambda e: e.tensor_copy(out=identb[:], in_=G.ident[:]), writes=["identb"])
        for s in range(2):
            P.pool(lambda e, s=s: e.memset(vst[s][:, :, 64:65], 1.0), writes=[("vst", s)])

        def norm_rope(src, sk, gain, gk, dst, dk, t, rope, w):
            sq_, hs_, R1_, R2_ = sq[w], hs[w], R1[w], R2[w]
            P.pool(lambda e: e.tensor_tensor(out=sq_[:], in0=src[:], in1=src[:], op=ALU.mult), reads=[sk],
                   writes=[("sq", w)])
            yield
            P.dve(lambda e: e.tensor_reduce(out=hs_[:, 0, :], in_=sq_[:], axis=AX, op=ALU.add), reads=[("sq", w)],
                  writes=[("hs", w)])
            yield
            P.dve(lambda e: e.tensor_scalar(out=hs_[:, 1, :], in0=hs_[:, 0, :], scalar1=1.0 / 96, scalar2=EPS,
                                            op0=ALU.mult, op1=ALU.add), reads=[("hs", w)], writes=[("hs", w)])
            yield
            P.act(lambda e: e.activation(out=hs_[:, 2, :], in_=hs_[:, 1, :], func=AF.Sqrt), reads=[("hs", w)],
                  writes=[("hs", w)])
            yield
            P.dve(lambda e: e.reciprocal(out=hs_[:, 3, :], in_=hs_[:, 2, :]), reads=[("hs", w)], writes=[("hs", w)])
            yield
            P.dve(lambda e: e.tensor_tensor(out=src[:], in0=src[:],
                                            in1=hs_[:, 3, :].unsqueeze(2).to_broadcast([128, 8, 96]), op=ALU.mult),
                  reads=[sk, ("hs", w)], writes=[sk])
            yield
            P.dve(lambda e: e.tensor_tensor(out=src[:], in0=src[:],
                                            in1=gain[:, :].unsqueeze(1).to_broadcast([128, 8, 96]), op=ALU.mult),
                  reads=[sk, gk], writes=[sk])
            yield
            if rope:
                P.dve(lambda e: e.tensor_tensor(out=R1_[:], in0=src[:, :, 64:96],
                                                in1=ropC[:, t, :].unsqueeze(1).to_broadcast([128, 8, 32]), op=ALU.mult),
                      reads=[sk, "rop"], writes=[("R1", w)])
                yield
                for (o0, i0) in ((0, 72), (8, 64), (16, 88), (24, 80)):
                    P.dve(lambda e, o0=o0, i0=i0: e.tensor_tensor(
                        out=R2_[:, :, o0:o0 + 8], in0=src[:, :, i0:i0 + 8],
                        in1=ropS[:, t, o0:o0 + 8].unsqueeze(1).to_broadcast([128, 8, 8]), op=ALU.mult),
                        reads=[sk, "rop"], writes=[("R2", w)])
                yield
                P.dve(lambda e: e.tensor_tensor(out=dst[:, :, 64:96], in0=R1_[:], in1=R2_[:], op=ALU.add),
                      reads=[("R1", w), ("R2", w)], writes=[dk])
                P.act(lambda e: e.activation(out=dst[:, :, 0:64], in_=src[:, :, 0:64], func=AF.Identity),
                      reads=[sk], writes=[dk])
                yield
            else:
                P.act(lambda e: e.activation(out=dst[:], in_=src[:], func=AF.Identity), reads=[sk], writes=[dk])
                yield

        def transpose_store(srcb, sbk, stt, stk, dram_view, semk, w):
            pk, pt = ("pst", w), ptb[w]
            for h in range(8):
                P.tr(pt[0:96, h * 128:(h + 1) * 128], srcb[:, h, :], identb[:], reads=[sbk, "identb"], writes=[pk])
            yield
            P.dve(lambda e: e.tensor_copy(out=stt[0:96, :, :], in_=pt[0:96, :].rearrange("p (h n) -> p h n", h=8)),
                  reads=[pk], writes=[stk])
            yield
            P.dma("sp", dram_view, stt[0:96, :, :], reads=[stk], writes=["dram_qk"], semkey=semk)
            yield

        def k_chain(t):
            s = t % 2
            w = 0
            rkv = mst[:, t, 4:5]
            pkk, psk = ("psm", 0), psf[0]
            P.mm(psk[:, :], [(ckt[s][:, k, :], wk[:, k, :]) for k in range(2)], reads=[("ckt", s), "wqkv"], writes=[pkk])
            pkv, psv = ("psm", 1), psf[1]
            P.mm(psv[:, :], [(ckt[s][:, k, :], wv[:, k, :]) for k in range(2)], reads=[("ckt", s), "wqkv"], writes=[pkv])
            yield
            P.act(lambda e: e.activation(out=vst[s][:, :, 0:64], in_=psv[:, :].rearrange("p (h e) -> p h e", h=8),
                                         func=AF.Identity, scale=rkv), reads=[pkv], writes=[("vst", s)])
            P.dma("sp", G.vs[t], vst[s][:], reads=[("vst", s)], writes=["dram_v"], semkey="vst%d" % s)
            yield
            P.act(lambda e: e.activation(out=kf[s][:, :, 0:64], in_=psk[:, :].rearrange("p (h e) -> p h e", h=8),
                                         func=AF.Identity, scale=rkv), reads=[pkk], writes=[("kf", s)])
            P.dve(lambda e: e.tensor_copy(out=kf[s][:, :, 64:96],
                                          in_=mst[:, t, 8:40].unsqueeze(1).to_broadcast([128, 8, 32])),
                  writes=[("kf", s)])
            yield
            yield from norm_rope(kf[s], ("kf", s), GK, "GK", xb[2 * w + s], ("xb", 2 * w + s), t, t < NT_L, 2 * w + s)
            yield from transpose_store(xb[2 * w + s], ("xb", 2 * w + s), xst[2 * w + s], ("xst", 2 * w + s),
                                       G.ks[:, :, t * 128:(t + 1) * 128].rearrange("h d n -> d h n"),
                                       "xst%d" % (2 * w + s), w)

        def q_chain(t):
            s = t % 2
            w = 1
            rq = mst[:, t, 3:4]
            qfl = qf[s][:].rearrange("p h e -> p (h e)")
            pka, psa = ("psm", 2), psf[2]
            P.mm(psa[:, :], [(cqt[s][:, k, :], wq[:, k, 0:512]) for k in range(6)], reads=[("cqt", s), "wqkv"],
                 writes=[pka])
            pkb, psb = ("psm", 3), psf[3]
            P.mm(psb[:, 0:256], [(cqt[s][:, k, :], wq[:, k, 512:768]) for k in range(6)],
                 reads=[("cqt", s), "wqkv"], writes=[pkb])
            yield
            P.act(lambda e: e.activation(out=qfl[:, 0:512], in_=psa[:, :], func=AF.Identity, scale=rq),
                  reads=[pka], writes=[("qf", s)])
            P.act(lambda e: e.activation(out=qfl[:, 512:768], in_=psb[:, 0:256], func=AF.Identity, scale=rq),
                  reads=[pkb], writes=[("qf", s)])
            yield
            yield from norm_rope(qf[s], ("qf", s), GQ, "GQ", xb[2 * w + s], ("xb", 2 * w + s), t, True, 2 * w + s)
            yield from transpose_store(xb[2 * w + s], ("xb", 2 * w + s), xst[2 * w + s], ("xst", 2 * w + s),
                                       G.qs[:, :, t * 128:(t + 1) * 128].rearrange("h d n -> d h n"),
                                       "xst%d" % (2 * w + s), w)

        def c1_loads(t):
            s = t % 2
            P.dma("pool", ckt[s][:], G.zs[10:12, :, t * 128:(t + 1) * 128].rearrange("j p n -> p j n"),
                  writes=[("ckt", s)], semkey="ckt%d" % s)
            if t < NT_L:
                P.dma("pool", cqt[s][:], G.zs[4:10, :, t * 128:(t + 1) * 128].rearrange("j p n -> p j n"),
                      writes=[("cqt", s)], semkey="cqt%d" % s)

        c1_loads(0)
        for t in range(NT):
            if t + 1 < NT:
                c1_loads(t + 1)
            run_zip([k_chain(t)] + ([q_chain(t)] if t < NT_L else []), 2)
        P.barrier()
        P.flush()
    with ExitStack() as c2:
        C = c2.enter_context
        qh = [C(_sb(nc, "qh%d" % s, [128, SEQ], BF16)) for s in range(2)]
        kh = [C(_sb(nc, "kh%d" % s, [128, T_ALL], BF16)) for s in range(2)]
        vh = [C(_sb(nc, "vh%d" % s, [128, NT, 65], BF16)) for s in range(2)]
        pT = [C(_sb(nc, "pT%d" % s, [128, 512], BF16)) for s in range(4)]
        otok = C(_sb(nc, "otok", [128, NT_L, 512], BF16))
        rec = C(_sb(nc, "rec", [128, 8], F32))
        identb = C(_sb(nc, "identb2", [128, 128], BF16))
        ost = [C(_sb(nc, "ost%d" % s, [128, 4, 128], BF16)) for s in range(2)]
        psS = [C(_ps(nc, "psS%d" % s, [128, 512], F32)) for s in range(3)]
        psO = [C(_ps(nc, "psO%d" % s, [128, 512], F32)) for s in range(4)]
        npt = _rot([C(_ps(nc, "pst2_%d" % s, [128, 1024], BF16)) for s in range(1)], "pst2")
        P.dve(lambda e: e.tensor_copy(out=identb[:], in_=G.ident[:]), writes=["identb"])
        steps = [(h, qb_, kc) for h in range(8) for qb_ in range(4) for kc in range(NT)]
        ri = [0]

        def head_loads(h):
            s = h % 2
            P.dma("sp", qh[s][0:96, :], G.qs[h], writes=[("qh", s)], semkey="qh%d" % s)
            P.dma("sp", kh[s][0:96, :], G.ks[h], writes=[("kh", s)], semkey="kh%d" % s)
            P.dma("sp", vh[s][:], G.vs[:, :, h, :].rearrange("t p e -> p t e"), writes=[("vh", s)],
                  semkey="vh%d" % s)

        def emit_S(i):
            h, qb_, kc = steps[i]
            s = h % 2
            if h == 0 and qb_ == 0 and kc == 0:
                head_loads(0)
            if qb_ == 0 and kc == 6 and h + 1 < 8:
                head_loads(h + 1)
            ss = i % 3
            P.mm(psS[ss][:, :], [(kh[s][0:96, kc * 128:(kc + 1) * 128], qh[s][0:96, qb_ * 512:(qb_ + 1) * 512])],
                 reads=[("qh", s), ("kh", s)], writes=[("psS", ss)])

        def emit_rest(i):
            h, qb_, kc = steps[i]
            s = h % 2
            ss = i % 3
            ps_ = i % 4
            P.act(lambda e: e.activation(out=pT[ps_][:], in_=psS[ss][:, :], func=AF.Exp),
                  reads=[("psS", ss)], writes=[("pT", ps_)])
            for j in range(4):
                wr = [("psO", j)] if kc in (0, NT - 1) else []
                Prog_mm1(P, psO[j][:, 0:65], pT[ps_][:, j * 128:(j + 1) * 128], vh[s][:, kc, :],
                         start=(kc == 0), stop=(kc == NT - 1), reads=[("pT", ps_), ("vh", s)], writes=wr)
            if kc == NT - 1:
                for j in range(4):
                    r_ = ri[0] % 8
                    ri[0] += 1
                    t = qb_ * 4 + j
                    P.dve(lambda e, j=j, r_=r_: e.reciprocal(out=rec[:, r_:r_ + 1], in_=psO[j][:, 64:65]),
                          reads=[("psO", j)], writes=[("rec", r_)])
                    P.act(lambda e, j=j, r_=r_, t=t, h=h: e.activation(out=otok[:, t, h * 64:(h + 1) * 64],
                                                                      in_=psO[j][:, 0:64], func=AF.Identity,
                                                                      scale=rec[:, r_:r_ + 1]),
                          reads=[("psO", j), ("rec", r_)], writes=["otok"])

        emit_S(0)
        emit_S(1)
        for i in range(len(steps)):
            if i + 2 < len(steps):
                emit_S(i + 2)
            emit_rest(i)
        for t in range(NT_L):
            s = t % 2
            pk, pt = npt()
            for c in range(4):
                P.tr(pt[:, c * 128:(c + 1) * 128], otok[:, t, c * 128:(c + 1) * 128], identb[:], reads=["otok", "identb"],
                     writes=[pk])
            P.dve(lambda e, s=s, pt=pt: e.tensor_copy(out=ost[s][:], in_=pt[:, 0:512].rearrange("p (c n) -> p c n", c=4)),
                  reads=[pk], writes=[("ost", s)])
            P.dma("sp", G.mixs[4:8, :, t * 128:(t + 1) * 128].rearrange("j p n -> p j n"), ost[s][:],
                  reads=[("ost", s)], writes=["mixs_o"], semkey="ost%d" % s)
        P.barrier()
        P.flush()
```
